# Optimizing a Trainium2 kernel written in Bass

```python
import math
import jax, jax.numpy as jnp
from jax import lax
import numpy as np

D_MODEL = 1024
BATCH = 4
SEQ = 8192
DEPTH = 1

N_HEADS_A = 8
HEAD_DIM_A = 64
N_IDX_HEADS = 8
IDX_DIM = 64
TOPK_MAX = 256
Q_BLOCK = 128
N_HEADS_B = 4
HEAD_DIM_B = 128
RET_CHUNK = 128
RET_THETA_BASE = 10000.0
D_A = N_HEADS_A * HEAD_DIM_A
D_B = N_HEADS_B * HEAD_DIM_B
D_MIX = D_A + D_B
D_FF = ((8 * D_MODEL // 3 + 255) // 256) * 256
N_BUCKETS = 32
MAX_DISTANCE = 128
EPS = 1e-6
SPLIT_SIZES = (D_A, D_A, D_A, N_IDX_HEADS * IDX_DIM, IDX_DIM, N_IDX_HEADS, D_B, D_B, D_B, D_B)
D_IN = D_A * 3 + N_IDX_HEADS * IDX_DIM + IDX_DIM + N_IDX_HEADS + D_B * 4

kernel_name = "hymba_dsa_retnet_hybrid_layer"


def rmsnorm(x, g):
    x32 = x.astype(jnp.float32)
    y = x32 * lax.rsqrt(jnp.mean(x32 * x32, axis=-1, keepdims=True) + EPS)
    return (y * g.astype(jnp.float32)).astype(x.dtype)


def layernorm(x, g, b):
    x32 = x.astype(jnp.float32)
    mu = jnp.mean(x32, axis=-1, keepdims=True)
    var = jnp.mean(jnp.square(x32 - mu), axis=-1, keepdims=True)
    y = (x32 - mu) * lax.rsqrt(var + EPS)
    return (y * g.astype(jnp.float32) + b.astype(jnp.float32)).astype(x.dtype)


def rel_bucket(dist):
    n = jnp.maximum(dist, 0)
    max_exact = N_BUCKETS // 2
    nf = jnp.maximum(n, max_exact).astype(jnp.float32)
    large = max_exact + (jnp.log(nf / max_exact) / math.log(MAX_DISTANCE / max_exact)
                         * (N_BUCKETS - max_exact)).astype(jnp.int32)
    large = jnp.minimum(large, N_BUCKETS - 1)
    return jnp.where(n < max_exact, n, large)


def rotate(x, pos):
    half = x.shape[-1] // 2
    theta = 1.0 / (RET_THETA_BASE ** jnp.linspace(0.0, 1.0, half, dtype=jnp.float32))
    ang = pos.astype(jnp.float32)[:, None] * theta[None, :]
    cos = jnp.cos(ang)[None, :, None, :]
    sin = jnp.sin(ang)[None, :, None, :]
    x32 = x.astype(jnp.float32)
    x1, x2 = x32[..., :half], x32[..., half:]
    out = jnp.concatenate([x1 * cos - x2 * sin, x2 * cos + x1 * sin], axis=-1)
    return out.astype(x.dtype)


def sparse_attention(q, k, v, q_idx, k_idx, w_idx, rel_bias, k_top):
    B, L = q.shape[0], q.shape[1]
    n_blocks = L // Q_BLOCK
    key_pos = jnp.arange(L, dtype=jnp.int32)
    b_ix = jnp.arange(B)[:, None, None]
    idx_scale = IDX_DIM ** -0.5
    attn_scale = HEAD_DIM_A ** -0.5

    def block(i):
        start = i * Q_BLOCK
        qi = lax.dynamic_slice_in_dim(q, start, Q_BLOCK, axis=1)
        qi_idx = lax.dynamic_slice_in_dim(q_idx, start, Q_BLOCK, axis=1)
        wi = lax.dynamic_slice_in_dim(w_idx, start, Q_BLOCK, axis=1)
        q_pos = start + jnp.arange(Q_BLOCK, dtype=jnp.int32)
        dots = jnp.einsum('bqhd,bsd->bqhs', qi_idx, k_idx) * idx_scale
        score = jnp.einsum('bqh,bqhs->bqs', wi, jax.nn.relu(dots)).astype(jnp.float32)
        causal = key_pos[None, :] <= q_pos[:, None]
        score = jnp.where(causal[None], score, -jnp.inf)
        _, sel = lax.top_k(score, k_top)
        k_sel = k[b_ix, sel]
        v_sel = v[b_ix, sel]
        logits = jnp.einsum('bqhd,bqkhd->bhqk', qi, k_sel).astype(jnp.float32) * attn_scale
        dist = q_pos[None, :, None] - sel
        bias = rel_bias[rel_bucket(dist)].astype(jnp.float32)
        logits = logits + jnp.transpose(bias, (0, 3, 1, 2))
        logits = jnp.where((dist >= 0)[:, None], logits, -jnp.inf)
        p = jax.nn.softmax(logits, axis=-1).astype(v.dtype)
        return jnp.einsum('bhqk,bqkhd->bqhd', p, v_sel)

    out = lax.map(block, jnp.arange(n_blocks))
    out = jnp.transpose(out, (1, 0, 2, 3, 4))
    return out.reshape(B, L, N_HEADS_A * HEAD_DIM_A)


def retention(q, k, v, g, gn_g):
    B, L, H, D = q.shape
    C = RET_CHUNK
    nc = L // C
    gamma = 1.0 - 2.0 ** (-5.0 - jnp.arange(H, dtype=jnp.float32))
    log_g = jnp.log(gamma)
    pos = jnp.arange(C, dtype=jnp.float32)
    diff = pos[:, None] - pos[None, :]
    inner_decay = jnp.where(diff >= 0, jnp.exp(log_g[:, None, None] * jnp.maximum(diff, 0.0)), 0.0)
    q_decay = jnp.exp(log_g[:, None] * (pos[None, :] + 1.0))
    k_decay = jnp.exp(log_g[:, None] * (C - 1.0 - pos[None, :]))
    chunk_decay = jnp.exp(log_g * C)
    dt = q.dtype
    inner_decay, q_decay, k_decay, chunk_decay = (a.astype(dt) for a in (inner_decay, q_decay, k_decay, chunk_decay))

    def to_chunks(t):
        return jnp.transpose(t.reshape(B, nc, C, H, D), (1, 0, 3, 2, 4))

    qc, kc, vc = to_chunks(q), to_chunks(k * (D ** -0.5)), to_chunks(v)

    def step(state, inp):
        qi, ki, vi = inp
        inner = jnp.einsum('bhid,bhjd->bhij', qi, ki) * inner_decay
        y = (jnp.einsum('bhij,bhje->bhie', inner, vi)
             + jnp.einsum('bhid,bhde->bhie', qi, state) * q_decay[None, :, :, None])
        state = (state * chunk_decay[None, :, None, None]
                 + jnp.einsum('bhjd,bhje->bhde', ki * k_decay[None, :, :, None], vi))
        return state, y

    state0 = jnp.zeros((B, H, D, D), dtype=dt)
    _, ys = lax.scan(step, state0, (qc, kc, vc))
    y = jnp.transpose(ys, (1, 0, 3, 2, 4)).reshape(B, L, H, D)
    y32 = y.astype(jnp.float32)
    y32 = y32 * lax.rsqrt(jnp.mean(y32 * y32, axis=-1, keepdims=True) + EPS)
    y = (y32.reshape(B, L, H * D) * gn_g.astype(jnp.float32)).astype(dt)
    return y * jax.nn.silu(g.reshape(B, L, H * D))


def setup_inputs(seed: int = 0) -> dict:
    key = jax.random.key(seed)
    ks = jax.random.split(key, 16)
    f32 = jnp.float32

    def nrm(k, shape, scale):
        return jax.random.normal(k, shape, f32) * scale

    return {
        "x": nrm(ks[0], (BATCH, SEQ, D_MODEL), 1.0),
        "norm_mix_g": 1.0 + nrm(ks[1], (DEPTH, D_MODEL), 0.02),
        "w_in": nrm(ks[2], (DEPTH, D_MODEL, D_IN), D_MODEL ** -0.5),
        "idx_k_ln_g": 1.0 + nrm(ks[3], (DEPTH, IDX_DIM), 0.02),
        "idx_k_ln_b": nrm(ks[4], (DEPTH, IDX_DIM), 0.02),
        "rel_bias": nrm(ks[5], (N_BUCKETS, N_HEADS_A), 0.5),
        "ret_gn_g": 1.0 + nrm(ks[6], (DEPTH, D_B), 0.02),
        "w_out": nrm(ks[7], (DEPTH, D_MIX, D_MODEL), D_MIX ** -0.5),
        "norm_ffn_g": 1.0 + nrm(ks[8], (DEPTH, D_MODEL), 0.02),
        "w_gate": nrm(ks[9], (DEPTH, D_MODEL, D_FF), D_MODEL ** -0.5),
        "w_up": nrm(ks[10], (DEPTH, D_MODEL, D_FF), D_MODEL ** -0.5),
        "w_down": nrm(ks[11], (DEPTH, D_FF, D_MODEL), D_FF ** -0.5),
        "norm_final_g": 1.0 + nrm(ks[12], (D_MODEL,), 0.02),
    }


def reference(x, norm_mix_g, w_in, idx_k_ln_g, idx_k_ln_b, rel_bias, ret_gn_g, w_out,
              norm_ffn_g, w_gate, w_up, w_down, norm_final_g):
    B, L, _ = x.shape
    k_top = min(TOPK_MAX, L // 4)
    pos = jnp.arange(L, dtype=jnp.int32)
    offsets = list(np.cumsum(SPLIT_SIZES)[:-1])
    for layer in range(DEPTH):
        h = rmsnorm(x, norm_mix_g[layer])
        proj = jnp.einsum('bld,de->ble', h, w_in[layer])
        (qa, ka, va, q_idx, k_idx, w_idx, qb, kb, vb, gb) = jnp.split(proj, offsets, axis=-1)
        qa = qa.reshape(B, L, N_HEADS_A, HEAD_DIM_A)
        ka = ka.reshape(B, L, N_HEADS_A, HEAD_DIM_A)
        va = va.reshape(B, L, N_HEADS_A, HEAD_DIM_A)
        q_idx = q_idx.reshape(B, L, N_IDX_HEADS, IDX_DIM)
        k_idx = layernorm(k_idx, idx_k_ln_g[layer], idx_k_ln_b[layer])
        w_idx = w_idx * (N_IDX_HEADS ** -0.5)
        out_a = sparse_attention(qa, ka, va, q_idx, k_idx, w_idx, rel_bias, k_top)
        qb = rotate(qb.reshape(B, L, N_HEADS_B, HEAD_DIM_B), pos)
        kb = rotate(kb.reshape(B, L, N_HEADS_B, HEAD_DIM_B), pos)
        vb = vb.reshape(B, L, N_HEADS_B, HEAD_DIM_B)
        gb = gb.reshape(B, L, N_HEADS_B, HEAD_DIM_B)
        out_b = retention(qb, kb, vb, gb, ret_gn_g[layer])
        mix = jnp.concatenate([out_a, out_b], axis=-1)
        x = x + jnp.einsum('ble,ed->bld', mix, w_out[layer])
        h2 = rmsnorm(x, norm_ffn_g[layer])
        u = jax.nn.silu(jnp.einsum('bld,df->blf', h2, w_gate[layer])) * jnp.einsum('bld,df->blf', h2, w_up[layer])
        x = x + jnp.einsum('blf,fd->bld', u, w_down[layer])
    return rmsnorm(x, norm_final_g)
```

```python
import math
from contextlib import ExitStack

import numpy as np
import concourse.bass as bass
import concourse.mybir as mybir
from concourse.bass_utils import run_bass_kernel_spmd

F32 = mybir.dt.float32
BF16 = mybir.dt.bfloat16
U8 = mybir.dt.uint8
ALU = mybir.AluOpType
AF = mybir.ActivationFunctionType
AX = mybir.AxisListType

D = 1024
DFF = 2816
NFC = DFF // 128
EPS = 1e-6
NEG = -1.0e30
MNEG = -30000.0
R_RANGE = 16.0
N_ITER = 18
GB = 2


class Buf:
    __slots__ = ("name", "w", "r", "dsem", "dcnt")

    def __init__(self, name):
        self.name = name
        self.w = None
        self.r = {}
        self.dsem = None
        self.dcnt = 0


class Eng:
    def __init__(self, name, obj):
        self.name = name
        self.obj = obj
        self.sem = None
        self.cnt = 0
        self.seen = {}


class Prog:
    EPOCH = 30000

    def __init__(self, nc, es):
        self.nc = nc
        self.es = es
        self.eng = {
            "pe": Eng("pe", nc.tensor),
            "act": Eng("act", nc.scalar),
            "dve": Eng("dve", nc.vector),
            "pool": Eng("pool", nc.gpsimd),
            "sp": Eng("sp", nc.sync),
        }
        self.nsem = 0
        self.ninst = 0
        self.nwait = 0
        self.allbufs = []
        self.oldsems = []
        for e in self.eng.values():
            e.sem = self.new_sem(e.name)

    def new_sem(self, name):
        self.nsem += 1
        return self.es.enter_context(self.nc.semaphore(f"s{self.nsem}_{name}"))

    def buf(self, name):
        b = Buf(name)
        self.allbufs.append(b)
        return b

    def _need(self, E, t, waits):
        if t is None:
            return
        sem, val = t
        k = id(sem)
        if E.seen.get(k, 0) >= val:
            return
        if E.name == "pe" and sem is E.sem:
            return
        if k in waits:
            if waits[k][1] < val:
                waits[k] = (sem, val)
        else:
            waits[k] = (sem, val)

    def _flush(self, E, waits):
        for k, (sem, val) in waits.items():
            for e2 in self.eng.values():
                if e2.sem is sem and val > e2.cnt:
                    raise RuntimeError(f"wait on pending ticket of {e2.name}: {val}>{e2.cnt} (on {E.name})")
            E.obj.wait_ge(sem, val)
            E.seen[k] = val
            self.nwait += 1

    def _emit_waits(self, E, reads, writes):
        waits = {}
        for b in reads:
            self._need(E, b.w, waits)
        for b in writes:
            self._need(E, b.w, waits)
            for t in b.r.values():
                self._need(E, t, waits)
        self._flush(E, waits)

    def _record(self, t, reads, writes):
        sem, val = t
        k = id(sem)
        for b in reads:
            old = b.r.get(k)
            if old is None or old[1] < val:
                b.r[k] = t
        for b in writes:
            b.w = t
            b.r = {}

    def op(self, en, fn, reads=(), writes=(), inc=True):
        E = self.eng[en]
        self._emit_waits(E, reads, writes)
        ins = fn()
        self.ninst += 1
        if inc:
            if E.cnt >= self.EPOCH:
                self.oldsems.append((E.sem, E.cnt))
                E.sem = self.new_sem(E.name)
                E.cnt = 0
            E.cnt += 1
            ins.then_inc(E.sem, 1)
            t = (E.sem, E.cnt)
        else:
            if E.cnt + 1 > self.EPOCH:
                raise RuntimeError("pending across epoch")
            t = (E.sem, E.cnt + 1)
        self._record(t, reads, writes)
        return ins

    def dma(self, en, out, in_, reads, writes, sembuf):
        E = self.eng[en]
        self._emit_waits(E, reads, writes)
        if sembuf.dsem is None:
            sembuf.dsem = self.new_sem("d_" + sembuf.name)
        ins = E.obj.dma_start(out=out, in_=in_)
        sembuf.dcnt += 16
        ins.then_inc(sembuf.dsem, 16)
        self.ninst += 1
        self._record((sembuf.dsem, sembuf.dcnt), reads, writes)
        return ins

    def barrier(self):
        for E in self.eng.values():
            waits = {}
            for e2 in self.eng.values():
                if e2 is not E and e2.cnt > 0:
                    self._need(E, (e2.sem, e2.cnt), waits)
            for b in self.allbufs:
                if b.dsem is not None and b.dcnt > 0:
                    self._need(E, (b.dsem, b.dcnt), waits)
            self._flush(E, waits)


def build(NP, TOPK, debug=False):
    L = 256 * NP
    NT = 128 * NP
    nc = bass.Bass("TRN2", target_bir_lowering=False)
    es0 = ExitStack()
    P = Prog(nc, es0)

    def din(name, shape, dt=F32):
        return nc.dram_tensor(name, list(shape), dt, kind="ExternalInput").ap()

    xo_d = din("xo", [NT, D])
    xt_d = din("xt", [NT, D])
    wk_d = din("wk", [D, 2112])
    wq_d = din("wq", [D, 2056])
    wo_d = din("wo", [D, D])
    wg_d = din("wg", [D, DFF])
    wu_d = din("wu", [D, DFF])
    wd_d = din("wd", [DFF, D])
    gmix_d = din("gmix", [128, 8])
    gffn_d = din("gffn", [128, 8])
    gfin_d = din("gfin", [128, D])
    lng_d = din("lng", [128, 64])
    lnb_d = din("lnb", [128, 64])
    gn_d = din("gn", [128, 512])
    b31_d = din("b31", [128, 8])
    bown_d = din("bown", [128, 8, 128])
    both_d = din("both", [128, 8, 128])
    coso_d = din("coso", [NT, 64])
    sino_d = din("sino", [NT, 64])
    cost_d = din("cost", [NT, 64])
    sint_d = din("sint", [NT, 64])
    decq_d = din("decq", [128, 4])
    deck_d = din("deck", [128, 4])
    gc_d = din("gc", [128, 4])
    ident_d = din("ident", [128, 128])
    tri_d = din("tri", [128, 128])
    kbias_d = din("kbias", [128, 128])
    trir_d = din("trir", [128, 128])
    out_d = nc.dram_tensor("out", [NT, D], F32, kind="ExternalOutput").ap()
    dbg_d = nc.dram_tensor("dbg", [128, 8192], F32, kind="ExternalOutput").ap() if debug else None
    KT_d = nc.dram_tensor("KTs", [128, 4, L], BF16, kind="Internal").ap()
    V_d = nc.dram_tensor("Vs", [2 * NP, 128, 520], BF16, kind="Internal").ap()
    MIX_d = nc.dram_tensor("MIXs", [NT, D], BF16, kind="Internal").ap()
    KT_b = [P.buf(f"KTd{j}") for j in range(2 * NP)]
    V_b = [P.buf(f"Vd{j}") for j in range(2 * NP)]
    MIX_b = [P.buf(f"MIXd{j}") for j in range(NP)]

    esp = ExitStack()
    def ps(name, shape, dt):
        return esp.enter_context(nc.psum_tensor("p_" + name, list(shape), dt))
    mm = [ps(f"mm{k}", [128, 512], F32) for k in range(2)]
    mm_b = [P.buf(f"mm{k}") for k in range(2)]
    tp = ps("tp", [128, 1024], BF16)
    tp_b = P.buf("tp")
    acc = [ps(f"acc{k}", [128, 512], F32) for k in range(2)]
    acc_b = [P.buf(f"acc{k}") for k in range(2)]
    rot = [ps(f"rot{k}", [128, 512], F32) for k in range(3)]
    rot_b = [P.buf(f"rot{k}") for k in range(3)]
    cnt = {"mm": 0, "rot": 0}

    def next_mm():
        k = cnt["mm"] % 2
        cnt["mm"] += 1
        return mm[k], mm_b[k]

    def next_rot():
        k = cnt["rot"] % 3
        cnt["rot"] += 1
        return rot[k], rot_b[k]

    def sb0(name, shape, dt):
        return es0.enter_context(nc.sbuf_tensor("s0_" + name, list(shape), dt))
    identf = sb0("identf", [128, 128], F32)
    identb = sb0("identb", [128, 128], BF16)
    mhalf = sb0("mhalf", [128, 8], F32)
    c_b = P.buf("consts")
    P.dma("sp", identf[:], ident_d[:, :], [], [c_b], c_b)
    P.op("pool", lambda: nc.gpsimd.memset(mhalf[:], -0.5), [], [c_b])
    P.op("pool", lambda: nc.gpsimd.tensor_copy(identb[:], identf[:]), [c_b], [c_b])

    dbg_state = {"off": 0}

    def rstd_from_ssq(ssq, ssq_b, rs, rs_b, n, width, tmp, tmp_b):
        P.op("dve", lambda: nc.vector.tensor_scalar(tmp, ssq, 1.0 / n, EPS, ALU.mult, ALU.add), [ssq_b], [tmp_b])
        P.op("pool", lambda: nc.gpsimd.tensor_tensor(rs, tmp, mhalf[:, 0:width], ALU.pow), [tmp_b, c_b], [rs_b])

    def load_weight_folded(dst, dst_b, src_d, ncols, gcol, gcol_b, stage, stage_b, CH):
        src_v = src_d.rearrange("(c p) n -> p c n", p=128)
        k = 0
        for n0 in range(0, ncols, CH):
            w = min(CH, ncols - n0)
            st, st_b = stage[k % 2], stage_b[k % 2]
            P.dma("sp", st[:, :, 0:w], src_v[:, :, n0:n0 + w], [], [st_b], st_b)
            for c in range(8):
                e = ("dve", "pool", "act")[(k * 8 + c) % 3]
                if e == "dve":
                    P.op("dve", lambda c=c: nc.vector.tensor_scalar(dst[:, c, n0:n0 + w], st[:, c, 0:w], gcol[:, c:c + 1], None, ALU.mult),
                         [st_b, gcol_b], [dst_b])
                elif e == "pool":
                    P.op("pool", lambda c=c: nc.gpsimd.tensor_scalar(dst[:, c, n0:n0 + w], st[:, c, 0:w], gcol[:, c:c + 1], 1.0, ALU.mult, ALU.mult),
                         [st_b, gcol_b], [dst_b])
                else:
                    P.op("act", lambda c=c: nc.scalar.activation(out=dst[:, c, n0:n0 + w], in_=st[:, c, 0:w], func=AF.Copy, scale=gcol[:, c:c + 1]),
                         [st_b, gcol_b], [dst_b])
            k += 1

    def norm_transpose(x_t, x_b, xb_t, xb_b, hT, hT_b, ssq, ssq_b, junk, junk_b, col_off):
        P.op("act", lambda: nc.scalar.activation(out=junk, in_=x_t, func=AF.Square, accum_out=ssq), [x_b], [junk_b, ssq_b])
        P.op("pool", lambda: nc.gpsimd.tensor_copy(xb_t, x_t), [x_b], [xb_b])
        for c in range(8):
            P.op("pe", lambda c=c: nc.tensor.transpose(tp[:, c * 128:(c + 1) * 128], xb_t[:, c * 128:(c + 1) * 128], identb[:]),
                 [xb_b, c_b], [tp_b], inc=(c == 7))
        P.op("dve", lambda: nc.vector.tensor_copy(hT[:, :, col_off:col_off + 128], tp[:, :].rearrange("p (c t) -> p c t", c=8)),
             [tp_b], [hT_b])

    es1 = ExitStack()
    def sb(name, shape, dt):
        return es1.enter_context(nc.sbuf_tensor("s1_" + name, list(shape), dt))

    Wk = sb("Wk", [128, 8, 2112], BF16); Wk_b = P.buf("Wk")
    Wq = sb("Wq", [128, 8, 2056], BF16); Wq_b = P.buf("Wq")
    kidxT = sb("kidxT", [64, L], BF16); kidxT_b = P.buf("kidxT")
    score = sb("score", [128, L], F32); score_b = P.buf("score")
    stage = [sb(f"stage{k}", [128, 8, 128], F32) for k in range(2)]
    stage_b = [P.buf(f"stage{k}") for k in range(2)]
    gmix = sb("gmix", [128, 8], F32)
    lng = sb("lng", [128, 64], F32); lnb = sb("lnb", [128, 64], F32)
    gn = sb("gn", [128, 512], F32)
    b31 = sb("b31", [128, 8], F32)
    bown = sb("bown", [128, 8, 128], BF16); both = sb("both", [128, 8, 128], BF16)
    decq = sb("decq", [128, 4], F32); deck = sb("deck", [128, 4], F32); gc = sb("gc", [128, 4], F32)
    tri = sb("tri", [128, 128], F32); kbias = sb("kbias", [128, 128], F32); trir = sb("trir", [128, 128], F32)
    k1_b = P.buf("consts1")
    for t_, d_ in ((gmix, gmix_d), (lng, lng_d), (lnb, lnb_d), (gn, gn_d), (b31, b31_d), (decq, decq_d),
                   (deck, deck_d), (gc, gc_d), (tri, tri_d), (kbias, kbias_d), (trir, trir_d)):
        P.dma("sp", t_[:], d_[:, :], [], [k1_b], k1_b)
    btmp, bt_b = stage[0], stage_b[0]
    for tab, tab_d in ((bown, bown_d), (both, both_d)):
        P.dma("sp", btmp[:], tab_d[:, :, :], [], [bt_b], bt_b)
        for h in range(8):
            P.op("dve", lambda h=h, tab=tab: nc.vector.tensor_scalar(tab[:, h, :], btmp[:, h, :], b31[:, h:h + 1], None, ALU.subtract),
                 [bt_b, k1_b], [k1_b])
    load_weight_folded(Wk, Wk_b, wk_d, 2112, gmix, k1_b, stage, stage_b, 128)
    load_weight_folded(Wq, Wq_b, wq_d, 2056, gmix, k1_b, stage, stage_b, 128)

    xo = sb("xo", [128, D], F32); xo_b = P.buf("xo")
    xt = sb("xt", [128, D], F32); xt_b = P.buf("xt")
    xbf = sb("xbf", [128, D], BF16); xbf_b = P.buf("xbf")
    hTo = sb("hTo", [128, 8, 128], BF16); hTo_b = P.buf("hTo")
    hTt, hTt_b = hTo, hTo_b
    cols = sb("cols", [128, 64], F32)
    colb = {k: P.buf("col_" + k) for k in ("ssq_o", "ssq_t", "rs_o", "rs_t", "tmp", "rsq8", "rsdq", "rsdk_o", "rsdk_t",
                                            "ln", "w", "ret", "bis", "den")}
    C_SSQ_O, C_SSQ_T, C_RS_O, C_RS_T, C_TMP, C_RSQ8 = 0, 1, 2, 3, 4, 5
    C_RSDQ, C_RSDK_O, C_RSDK_T = 8, 12, 16
    C_LN = 20
    C_W = 28
    C_RET = 36
    C_BIS = 48
    C_DEN = 56
    cos_o = sb("cos_o", [128, 64], F32); sin_o = sb("sin_o", [128, 64], F32)
    cos_t = sb("cos_t", [128, 64], F32); sin_t = sb("sin_t", [128, 64], F32)
    tab_b = P.buf("rot_tabs")
    ka_tok = sb("ka_tok", [128, 512], BF16); ka_tok_b = P.buf("ka_tok")
    kaT_blk = [sb(f"kaT_blk{k}", [128, 4, 128], BF16) for k in range(2)]; kaT_blk_b = [P.buf(f"kaT_blk{k}") for k in range(2)]
    va_tok = [sb(f"va_tok{k}", [128, 8, 65], BF16) for k in range(2)]; va_tok_b = [P.buf(f"va_tok{k}") for k in range(2)]
    kx = sb("kx", [128, 64], F32); kx_b = P.buf("kx")
    kc = sb("kc", [128, 64], F32); kc_b = P.buf("kc")
    kn = sb("kn", [128, 64], BF16); kn_b = P.buf("kn")
    ftmp = sb("ftmp", [128, 512], F32); ftmp_b = P.buf("ftmp")
    rt1 = sb("rt1", [128, 4, 64], F32); rt2 = sb("rt2", [128, 4, 64], F32); rt1_b = P.buf("rt1"); rt2_b = P.buf("rt2")
    Kp_t = sb("Kp_t", [128, 512], BF16); Kp_t_b = P.buf("Kp_t")
    Kp_o = sb("Kp_o", [128, 512], BF16); Kp_o_b = P.buf("Kp_o")
    KpT = sb("KpT", [128, 4, 128], BF16); KpT_b = P.buf("KpT")
    Vb_t = sb("Vb_t", [128, 512], BF16); Vb_t_b = P.buf("Vb_t")
    Vb_o = sb("Vb_o", [128, 512], BF16); Vb_o_b = P.buf("Vb_o")
    qa_tok = sb("qa_tok", [128, 512], BF16); qa_tok_b = P.buf("qa_tok")
    qaT = sb("qaT", [128, 4, 128], BF16); qaT_b = P.buf("qaT")
    qi_tok = sb("qi_tok", [128, 512], BF16); qi_tok_b = P.buf("qi_tok")
    qiT = sb("qiT", [64, 8, 128], BF16); qiT_b = P.buf("qiT")
    Qp = sb("Qp", [128, 512], BF16); Qp_b = P.buf("Qp")
    QpT = sb("QpT", [128, 4, 128], BF16); QpT_b = P.buf("QpT")
    gsil = sb("gsil", [128, 512], F32); gsil_b = P.buf("gsil")
    diag = sb("diag", [128, 8, 128], BF16); diag_b = P.buf("diag")
    Rh = [sb(f"Rh{h}", [128, 512], BF16) for h in range(8)]; Rh_b = [P.buf(f"Rh{h}") for h in range(8)]
    def n_act_of(nk_):
        return ((nk_ * 55 // 100) // 128) * 128
    NACT_MAX = max(1024, n_act_of(L))
    NDVE_MAX = max((2 * i_ + 2) * 128 - n_act_of((2 * i_ + 2) * 128) for i_ in range(NP))
    junk_a = sb("junk_a", [128, NACT_MAX], U8); junk_a_b = P.buf("junk_a")
    junk_d = sb("junk_d", [128, NDVE_MAX], U8); junk_d_b = P.buf("junk_d")
    junkx, junkx_b = junk_a, junk_a_b
    AB = 2
    mk = [sb(f"mk{k}", [128, AB * 128], BF16) for k in range(2)]; mk_b = [P.buf(f"mk{k}") for k in range(2)]
    mkT = [sb(f"mkT{k}", [128, AB, 128], BF16) for k in range(2)]; mkT_b = [P.buf(f"mkT{k}") for k in range(2)]
    KTs = [sb(f"KTs{k}", [128, 4, AB * 128], BF16) for k in range(2)]; KTs_b = [P.buf(f"KTs{k}") for k in range(2)]
    Vs = [sb(f"Vs{k}", [128, AB, 520], BF16) for k in range(2)]; Vs_b = [P.buf(f"Vs{k}") for k in range(2)]
    PT = [sb(f"PT{k}", [128, 2, 4, 128], BF16) for k in range(2)]; PT_b = [[P.buf(f"PT{k}_{r}") for r in range(2)] for k in range(2)]
    S = sb("S", [128, 4, 128], F32); S_b = P.buf("S")
    Sb = sb("Sb", [128, 4, 128], BF16); Sb_b = P.buf("Sb")
    inTD = sb("inTD", [128, 4, 128], BF16); inTD_b = P.buf("inTD")
    ytmp, ytmp_b = ftmp, ftmp_b
    stmp, stmp_b = ftmp[:, :].rearrange("p (h e) -> p h e", h=4), ftmp_b
    mix = sb("mix", [128, D], BF16); mix_b = P.buf("mix")
    rec = sb("rec", [128, 8], F32); rec_b = P.buf("rec")

    P.op("dve", lambda: nc.vector.memset(S[:], 0.0), [], [S_b])
    P.op("dve", lambda: nc.vector.memset(Sb[:], 0.0), [], [Sb_b])
    for k in range(2):
        P.op("pool", lambda k=k: nc.gpsimd.memset(va_tok[k][:], 1.0), [], [va_tok_b[k]])

    def col(c, w=1):
        return cols[:, c:c + w]

    def evac_scaled(eng_sel, dst, src, scale_col, scale_b, dst_b, src_b):
        if eng_sel == "act":
            P.op("act", lambda: nc.scalar.activation(out=dst, in_=src, func=AF.Copy, scale=scale_col), [src_b, scale_b], [dst_b])
        else:
            P.op("dve", lambda: nc.vector.tensor_scalar(dst, src, scale_col, None, ALU.mult), [src_b, scale_b], [dst_b])

    def project(W, W_b, c0, ncols, hT, hT_b):
        pt, pb = next_mm()
        for c in range(8):
            P.op("pe", lambda c=c: nc.tensor.matmul(pt[:, 0:ncols], hT[:, c, :], W[:, c, c0:c0 + ncols], start=(c == 0), stop=(c == 7)),
                 [hT_b, W_b], [pb], inc=(c == 7))
        return pt, pb

    def rotate(dst, dst_b, src3, cos_t_, sin_t_, src_b):
        cb = cos_t_[:, :].unsqueeze(1).to_broadcast([128, 4, 64])
        sbb = sin_t_[:, :].unsqueeze(1).to_broadcast([128, 4, 64])
        x1 = src3[:, :, 0:64]
        x2 = src3[:, :, 64:128]
        P.op("dve", lambda: nc.vector.tensor_tensor(rt1[:], x1, cb, ALU.mult), [src_b, tab_b], [rt1_b])
        P.op("pool", lambda: nc.gpsimd.tensor_tensor(rt2[:], x2, sbb, ALU.mult), [src_b, tab_b], [rt2_b])
        P.op("dve", lambda: nc.vector.tensor_tensor(dst[:, :, 0:64], rt1[:], rt2[:], ALU.subtract), [rt1_b, rt2_b], [dst_b])
        P.op("dve", lambda: nc.vector.tensor_tensor(rt1[:], x2, cb, ALU.mult), [src_b, tab_b], [rt1_b])
        P.op("pool", lambda: nc.gpsimd.tensor_tensor(rt2[:], x1, sbb, ALU.mult), [src_b, tab_b], [rt2_b])
        P.op("dve", lambda: nc.vector.tensor_tensor(dst[:, :, 64:128], rt1[:], rt2[:], ALU.add), [rt1_b, rt2_b], [dst_b])

    def kside(lb, hT, hT_b, rs_c, rs_b, rsdk_c, rsdk_b, cos_t_, sin_t_, Kp, Kp_b, Vb, Vb_b):
        slot = lb % 2
        pt, pb = project(Wk, Wk_b, 0, 512, hT, hT_b)
        evac_scaled("act", ka_tok[:], pt[:, :], col(rs_c), rs_b, ka_tok_b, pb)
        for c in range(4):
            P.op("pe", lambda c=c: nc.tensor.transpose(tp[:, c * 128:(c + 1) * 128], ka_tok[:, c * 128:(c + 1) * 128], identb[:]),
                 [ka_tok_b, c_b], [tp_b], inc=(c == 3))
        P.op("dve", lambda: nc.vector.tensor_copy(kaT_blk[slot][:], tp[:, 0:512].rearrange("p (c t) -> p c t", c=4)),
             [tp_b], [kaT_blk_b[slot]])
        P.dma("sp", KT_d[:, :, lb * 128:(lb + 1) * 128], kaT_blk[slot][:], [kaT_blk_b[slot]], [KT_b[lb]], kaT_blk_b[slot])
        pt, pb = project(Wk, Wk_b, 512, 512, hT, hT_b)
        evac_scaled("dve", va_tok[slot][:, :, 0:64], pt[:, :].rearrange("p (h d) -> p h d", h=8), col(rs_c), rs_b, va_tok_b[slot], pb)
        P.dma("sp", V_d[lb, :, :], va_tok[slot][:].rearrange("p h d -> p (h d)"), [va_tok_b[slot]], [V_b[lb]], va_tok_b[slot])
        pt, pb = project(Wk, Wk_b, 1024, 512, hT, hT_b)
        for h in range(4):
            evac_scaled("act" if h % 2 == 0 else "dve", ftmp[:, h * 128:(h + 1) * 128], pt[:, h * 128:(h + 1) * 128],
                        col(rsdk_c + h), rsdk_b, ftmp_b, pb)
        rotate(Kp[:, :].rearrange("p (h d) -> p h d", h=4), Kp_b, ftmp[:, :].rearrange("p (h d) -> p h d", h=4), cos_t_, sin_t_, ftmp_b)
        pt, pb = project(Wk, Wk_b, 1536, 512, hT, hT_b)
        evac_scaled("act", Vb[:], pt[:, :], col(rs_c), rs_b, Vb_b, pb)
        pt, pb = project(Wk, Wk_b, 2048, 64, hT, hT_b)
        lb_ = colb["ln"]
        P.op("act", lambda: nc.scalar.activation(out=kx[:], in_=pt[:, 0:64], func=AF.Copy, scale=col(rs_c), accum_out=col(C_LN)),
             [pb, rs_b], [kx_b, lb_])
        P.op("dve", lambda: nc.vector.tensor_scalar(col(C_LN + 1), col(C_LN), -1.0 / 64, None, ALU.mult), [lb_], [lb_])
        P.op("act", lambda: nc.scalar.activation(out=kc[:], in_=kx[:], func=AF.Square, bias=col(C_LN + 1), accum_out=col(C_LN + 2)),
             [kx_b, lb_], [kc_b, lb_])
        P.op("dve", lambda: nc.vector.tensor_scalar(col(C_LN + 3), col(C_LN + 2), 1.0 / 64, EPS, ALU.mult, ALU.add), [lb_], [lb_])
        P.op("pool", lambda: nc.gpsimd.tensor_tensor(col(C_LN + 4), col(C_LN + 3), mhalf[:, 0:1], ALU.pow), [lb_, c_b], [lb_])
        P.op("dve", lambda: nc.vector.tensor_scalar(kc[:], kx[:], col(C_LN + 1), col(C_LN + 4), ALU.add, ALU.mult), [kx_b, lb_], [kc_b])
        P.op("dve", lambda: nc.vector.tensor_tensor(kc[:], kc[:], lng[:], ALU.mult), [kc_b, k1_b], [kc_b])
        P.op("dve", lambda: nc.vector.tensor_tensor(kn[:], kc[:], lnb[:], ALU.add), [kc_b, k1_b], [kn_b])
        P.op("pe", lambda: nc.tensor.transpose(tp[0:64, 0:128], kn[:, :], identb[:]), [kn_b, c_b], [tp_b])
        P.op("act", lambda: nc.scalar.copy(kidxT[:, lb * 128:(lb + 1) * 128], tp[0:64, 0:128]), [tp_b], [kidxT_b])

    def state_update(Kp, Kp_b, Vb, Vb_b):
        pt, pb = next_mm()
        for h in range(4):
            P.op("pe", lambda h=h: nc.tensor.matmul(pt[:, h * 128:(h + 1) * 128], Kp[:, h * 128:(h + 1) * 128], Vb[:, h * 128:(h + 1) * 128],
                                                    start=(h == 0), stop=(h == 3)), [Kp_b, Vb_b], [pb], inc=(h == 3))
        P.op("dve", lambda: nc.vector.tensor_tensor(stmp, pt[:, :].rearrange("p (h e) -> p h e", h=4), S[:], ALU.add), [pb, S_b], [stmp_b])
        P.op("dve", lambda: nc.vector.tensor_tensor(S[:], stmp, gc[:, :].unsqueeze(2).to_broadcast([128, 4, 128]), ALU.mult),
             [stmp_b, k1_b], [S_b])
        P.op("act", lambda: nc.scalar.copy(Sb[:], S[:]), [S_b], [Sb_b])

    def dbg_dump(src, src_b, width):
        if not debug:
            return
        o = dbg_state["off"]
        tmpd = sb(f"dbgt{o}", [128, width], F32)
        tb_ = P.buf(f"dbgt{o}")
        P.op("dve", lambda: nc.vector.tensor_copy(tmpd[:], src), [src_b], [tb_])
        P.dma("sp", dbg_d[:, o:o + width], tmpd[:], [tb_], [], tb_)
        dbg_state["off"] = o + width

    for i in range(NP):
        lo_t, lo_o = 2 * i, 2 * i + 1
        rows = slice(i * 128, (i + 1) * 128)
        P.dma("sp", xt[:], xt_d[rows, :], [], [xt_b], xt_b)
        P.dma("sp", xo[:], xo_d[rows, :], [], [xo_b], xo_b)
        for t_, d_ in ((cos_o, coso_d), (sin_o, sino_d), (cos_t, cost_d), (sin_t, sint_d)):
            P.dma("sp", t_[:], d_[rows, :], [], [tab_b], tab_b)
        norm_transpose(xt[:], xt_b, xbf[:], xbf_b, hTt, hTt_b, col(C_SSQ_T), colb["ssq_t"], junkx[:, 0:D], junkx_b, 0)
        rstd_from_ssq(col(C_SSQ_T), colb["ssq_t"], col(C_RS_T), colb["rs_t"], D, 1, col(C_TMP), colb["tmp"])
        P.op("dve", lambda: nc.vector.tensor_scalar(col(C_RSDK_T, 4), deck[:], col(C_RS_T), None, ALU.mult), [k1_b, colb["rs_t"]], [colb["rsdk_t"]])
        kside(lo_t, hTt, hTt_b, C_RS_T, colb["rs_t"], C_RSDK_T, colb["rsdk_t"], cos_t, sin_t, Kp_t, Kp_t_b, Vb_t, Vb_t_b)
        norm_transpose(xo[:], xo_b, xbf[:], xbf_b, hTo, hTo_b, col(C_SSQ_O), colb["ssq_o"], junkx[:, 0:D], junkx_b, 0)
        rstd_from_ssq(col(C_SSQ_O), colb["ssq_o"], col(C_RS_O), colb["rs_o"], D, 1, col(C_TMP), colb["tmp"])
        P.op("dve", lambda: nc.vector.tensor_scalar(col(C_RSDK_O, 4), deck[:], col(C_RS_O), None, ALU.mult), [k1_b, colb["rs_o"]], [colb["rsdk_o"]])
        P.op("dve", lambda: nc.vector.tensor_scalar(col(C_RSDQ, 4), decq[:], col(C_RS_O), None, ALU.mult), [k1_b, colb["rs_o"]], [colb["rsdq"]])
        P.op("dve", lambda: nc.vector.tensor_scalar(col(C_RSQ8), col(C_RS_O), 0.125, None, ALU.mult), [colb["rs_o"]], [colb["rsq8"]])
        kside(lo_o, hTo, hTo_b, C_RS_O, colb["rs_o"], C_RSDK_O, colb["rsdk_o"], cos_o, sin_o, Kp_o, Kp_o_b, Vb_o, Vb_o_b)
        for h in range(4):
            P.op("pe", lambda h=h: nc.tensor.transpose(tp[:, h * 128:(h + 1) * 128], Kp_o[:, h * 128:(h + 1) * 128], identb[:]),
                 [Kp_o_b, c_b], [tp_b], inc=(h == 3))
        P.op("act", lambda: nc.scalar.copy(KpT[:], tp[:, 0:512].rearrange("p (h t) -> p h t", h=4)), [tp_b], [KpT_b])
        pt, pb = project(Wq, Wq_b, 0, 512, hTo, hTo_b)
        evac_scaled("act", qa_tok[:], pt[:, :], col(C_RSQ8), colb["rsq8"], qa_tok_b, pb)
        for c in range(4):
            P.op("pe", lambda c=c: nc.tensor.transpose(tp[:, c * 128:(c + 1) * 128], qa_tok[:, c * 128:(c + 1) * 128], identb[:]),
                 [qa_tok_b, c_b], [tp_b], inc=(c == 3))
        P.op("dve", lambda: nc.vector.tensor_copy(qaT[:], tp[:, 0:512].rearrange("p (c t) -> p c t", c=4)), [tp_b], [qaT_b])
        pt, pb = project(Wq, Wq_b, 512, 512, hTo, hTo_b)
        evac_scaled("dve", qi_tok[:], pt[:, :], col(C_RSQ8), colb["rsq8"], qi_tok_b, pb)
        for h in range(8):
            P.op("pe", lambda h=h: nc.tensor.transpose(tp[0:64, h * 128:(h + 1) * 128], qi_tok[:, h * 64:(h + 1) * 64], identb[:]),
                 [qi_tok_b, c_b], [tp_b], inc=(h == 7))
        P.op("act", lambda: nc.scalar.copy(qiT[:], tp[0:64, :].rearrange("p (h t) -> p h t", h=8)), [tp_b], [qiT_b])
        pt, pb = project(Wq, Wq_b, 1024, 512, hTo, hTo_b)
        for h in range(4):
            evac_scaled("act" if h % 2 == 0 else "dve", ftmp[:, h * 128:(h + 1) * 128], pt[:, h * 128:(h + 1) * 128],
                        col(C_RSDQ + h), colb["rsdq"], ftmp_b, pb)
        rotate(Qp[:, :].rearrange("p (h d) -> p h d", h=4), Qp_b, ftmp[:, :].rearrange("p (h d) -> p h d", h=4), cos_o, sin_o, ftmp_b)
        for h in range(4):
            P.op("pe", lambda h=h: nc.tensor.transpose(tp[:, h * 128:(h + 1) * 128], Qp[:, h * 128:(h + 1) * 128], identb[:]),
                 [Qp_b, c_b], [tp_b], inc=(h == 3))
        P.op("dve", lambda: nc.vector.tensor_copy(QpT[:], tp[:, 0:512].rearrange("p (h t) -> p h t", h=4)), [tp_b], [QpT_b])
        pt, pb = project(Wq, Wq_b, 1536, 512, hTo, hTo_b)
        P.op("act", lambda: nc.scalar.activation(out=gsil[:], in_=pt[:, :], func=AF.Silu, scale=col(C_RS_O)), [pb, colb["rs_o"]], [gsil_b])
        pt, pb = project(Wq, Wq_b, 2048, 8, hTo, hTo_b)
        P.op("dve", lambda: nc.vector.tensor_scalar(col(C_W, 8), pt[:, 0:8], col(C_RS_O), 8.0 ** -0.5, ALU.mult, ALU.mult),
             [pb, colb["rs_o"]], [colb["w"]])
        for h in range(8):
            P.op("pool", lambda h=h: nc.gpsimd.tensor_scalar(diag[:, h, :], identf[:], col(C_W + h), 1.0, ALU.mult, ALU.mult),
                 [c_b, colb["w"]], [diag_b])

        state_update(Kp_t, Kp_t_b, Vb_t, Vb_t_b)
        pt, pb = next_mm()
        for h in range(4):
            P.op("pe", lambda h=h: nc.tensor.matmul(pt[:, h * 128:(h + 1) * 128], KpT[:, h, :], QpT[:, h, :], start=(h == 0), stop=(h == 3)),
                 [KpT_b, QpT_b], [pb], inc=(h == 3))
        P.op("dve", lambda: nc.vector.tensor_tensor(inTD[:], pt[:, :].rearrange("p (h t) -> p h t", h=4),
                                                    trir[:, :].unsqueeze(1).to_broadcast([128, 4, 128]), ALU.mult), [pb, k1_b], [inTD_b])
        py, pyb = next_mm()
        for h in range(4):
            P.op("pe", lambda h=h: nc.tensor.matmul(py[:, h * 128:(h + 1) * 128], inTD[:, h, :], Vb_o[:, h * 128:(h + 1) * 128],
                                                    start=(h == 0), stop=False), [inTD_b, Vb_o_b], [pyb], inc=False)
        for h in range(4):
            P.op("pe", lambda h=h: nc.tensor.matmul(py[:, h * 128:(h + 1) * 128], QpT[:, h, :], Sb[:, h, :], start=False, stop=(h == 3)),
                 [QpT_b, Sb_b], [pyb], inc=(h == 3))
        rb_ = colb["ret"]
        for h in range(4):
            P.op("act", lambda h=h: nc.scalar.activation(out=ytmp[:, h * 128:(h + 1) * 128], in_=py[:, h * 128:(h + 1) * 128], func=AF.Square,
                                                         accum_out=col(C_RET + h)), [pyb], [ytmp_b, rb_])
        rstd_from_ssq(col(C_RET, 4), rb_, col(C_RET + 8, 4), rb_, 128, 4, col(C_RET + 4, 4), rb_)
        for h in range(4):
            P.op("dve", lambda h=h: nc.vector.scalar_tensor_tensor(out=ytmp[:, h * 128:(h + 1) * 128], in0=py[:, h * 128:(h + 1) * 128],
                                                                   scalar=col(C_RET + 8 + h), in1=gn[:, h * 128:(h + 1) * 128],
                                                                   op0=ALU.mult, op1=ALU.mult), [pyb, rb_, k1_b], [ytmp_b])
        P.op("dve", lambda: nc.vector.tensor_tensor(mix[:, 512:1024], ytmp[:], gsil[:], ALU.mult), [ytmp_b, gsil_b], [mix_b])
        state_update(Kp_o, Kp_o_b, Vb_o, Vb_o_b)

        nblk = 2 * i + 2
        nk = nblk * 128
        nslab = (nblk + 3) // 4
        for s in range(nslab):
            b0 = 4 * s
            nb = min(4, nblk - b0)
            ncol = nb * 128
            k0 = b0 * 128
            pacc, pacc_b = acc[s % 2], acc_b[s % 2]
            pend = []
            for h in range(8):
                pr, prb = next_rot()
                P.op("pe", lambda h=h, pr=pr: nc.tensor.matmul(pr[:, 0:ncol], qiT[:, h, :], kidxT[:, k0:k0 + ncol], start=True, stop=True),
                     [qiT_b, kidxT_b], [prb])
                if h % 2 == 0:
                    P.op("act", lambda h=h, pr=pr: nc.scalar.activation(out=Rh[h][:, 0:ncol], in_=pr[:, 0:ncol], func=AF.Relu), [prb], [Rh_b[h]])
                else:
                    P.op("dve", lambda h=h, pr=pr: nc.vector.tensor_scalar(Rh[h][:, 0:ncol], pr[:, 0:ncol], 0.0, None, ALU.max), [prb], [Rh_b[h]])
                pend.append(h)
                if len(pend) > 2:
                    hh = pend.pop(0)
                    P.op("pe", lambda hh=hh: nc.tensor.matmul(pacc[:, 0:ncol], diag[:, hh, :], Rh[hh][:, 0:ncol], start=(hh == 0), stop=False),
                         [diag_b, Rh_b[hh]], [pacc_b], inc=False)
            for hh in pend:
                P.op("pe", lambda hh=hh: nc.tensor.matmul(pacc[:, 0:ncol], diag[:, hh, :], Rh[hh][:, 0:ncol], start=(hh == 0), stop=(hh == 7)),
                     [diag_b, Rh_b[hh]], [pacc_b], inc=(hh == 7))
            c_lo, c_hi = 0, ncol
            if s == 0:
                P.op("dve", lambda: nc.vector.tensor_tensor(score[:, 0:128], pacc[:, 0:128], kbias[:], ALU.add), [pacc_b, k1_b], [score_b])
                c_lo = 128
            if s == nslab - 1:
                P.op("dve", lambda: nc.vector.tensor_tensor(score[:, k0 + ncol - 128:k0 + ncol], pacc[:, ncol - 128:ncol], tri[:], ALU.add),
                     [pacc_b, k1_b], [score_b])
                c_hi = ncol - 128
            if c_hi > c_lo:
                P.op("act", lambda: nc.scalar.copy(score[:, k0 + c_lo:k0 + c_hi], pacc[:, c_lo:c_hi]), [pacc_b], [score_b])

        bb = colb["bis"]
        TR, CN, SG, TT, GG, THR = C_BIS, C_BIS + 1, C_BIS + 2, C_BIS + 3, C_BIS + 4, C_BIS + 5
        n_act = n_act_of(nk)
        n_dve = nk - n_act
        step0 = R_RANGE
        P.op("dve", lambda: nc.vector.memset(col(TR), -R_RANGE + step0), [], [bb])
        for it in range(N_ITER):
            step_next = R_RANGE / (2.0 ** (it + 1))
            if n_act > 0:
                P.op("act", lambda: nc.scalar.activation(out=junk_a[:, 0:n_act], in_=score[:, 0:n_act], func=AF.Sign, bias=col(TR), scale=-1.0,
                                                         accum_out=col(SG)), [score_b, bb], [junk_a_b, bb])
            P.op("dve", lambda: nc.vector.tensor_scalar(junk_d[:, 0:n_dve], score[:, n_act:nk], col(TR), None, ALU.is_ge, ALU.add, accum_out=col(CN)),
                 [score_b, bb], [junk_d_b, bb])
            if n_act > 0:
                P.op("dve", lambda: nc.vector.scalar_tensor_tensor(out=col(TT), in0=col(CN), scalar=2.0, in1=col(SG), op0=ALU.mult, op1=ALU.subtract),
                     [bb], [bb])
                thresh = 2.0 * TOPK - n_act - 1.0
            else:
                P.op("dve", lambda: nc.vector.tensor_copy(col(TT), col(CN)), [bb], [bb])
                thresh = TOPK - 0.5
            P.op("dve", lambda: nc.vector.tensor_scalar(col(GG), col(TT), thresh, 2.0 * step_next, ALU.is_ge, ALU.mult), [bb], [bb])
            P.op("dve", lambda: nc.vector.scalar_tensor_tensor(out=col(TR), in0=col(GG), scalar=-step_next, in1=col(TR), op0=ALU.add, op1=ALU.add),
                 [bb], [bb])
        step_last = R_RANGE / (2.0 ** N_ITER)
        P.op("dve", lambda: nc.vector.tensor_scalar(col(THR), col(TR), -step_last, None, ALU.add), [bb], [bb])

        oacc, oacc_b = acc, acc_b
        first_pv = [True, True]
        for s in range(nblk // AB):
            b0 = AB * s
            nb = AB
            ncol = nb * 128
            k0 = b0 * 128
            sl = s % 2
            P.dma("sp", KTs[sl][:, :, 0:ncol], KT_d[:, :, k0:k0 + ncol], [KT_b[b0 + j] for j in range(nb)], [KTs_b[sl]], KTs_b[sl])
            for j in range(nb):
                P.dma("sp", Vs[sl][:, j, :], V_d[b0 + j, :, :], [V_b[b0 + j]], [Vs_b[sl]], Vs_b[sl])
            P.op("dve", lambda: nc.vector.tensor_scalar(mk[sl][:, 0:ncol], score[:, k0:k0 + ncol], col(THR), MNEG, ALU.is_lt, ALU.mult),
                 [score_b, bb], [mk_b[sl]])
            for j in range(nb):
                P.op("pe", lambda j=j: nc.tensor.transpose(tp[:, j * 128:(j + 1) * 128], mk[sl][:, j * 128:(j + 1) * 128], identb[:]),
                     [mk_b[sl], c_b], [tp_b], inc=(j == nb - 1))
            P.op("act", lambda: nc.scalar.copy(mkT[sl][:, 0:nb, :], tp[:, 0:ncol].rearrange("p (j t) -> p j t", j=nb)), [tp_b], [mkT_b[sl]])
            for j in range(nb):
                lbk = b0 + j
                near = None
                if lbk == nblk - 1:
                    near = bown
                elif lbk == nblk - 2:
                    near = both
                pk = (s * AB + j) % 2
                for par in range(2):
                    pr, prb = next_rot()
                    prv = pr[:, :].rearrange("p (c t) -> p c t", c=4)
                    for c in range(4):
                        lo = 64 * par
                        P.op("pe", lambda c=c, lo=lo, prv=prv: nc.tensor.matmul(prv[:, c, :], KTs[sl][lo:lo + 64, c, j * 128:(j + 1) * 128],
                                                                                qaT[lo:lo + 64, c, :], start=(c == 0), stop=False),
                             [KTs_b[sl], qaT_b], [prb], inc=False)
                    if near is not None:
                        nv = near[:, :, :].rearrange("p (c two) t -> p c two t", two=2)[:, :, par, :]
                        P.op("pe", lambda prv=prv, nv=nv: nc.tensor.matmul(prv, identb[:], nv, start=False, stop=False), [k1_b, c_b], [prb], inc=False)
                    mrep = mkT[sl][:, j, :].unsqueeze(1).to_broadcast([128, 4, 128])
                    P.op("pe", lambda prv=prv, mrep=mrep: nc.tensor.matmul(prv, identb[:], mrep, start=False, stop=True), [mkT_b[sl], c_b], [prb])
                    P.op("act", lambda pr=pr, par=par: nc.scalar.activation(out=PT[pk][:, par, :, :].rearrange("p c t -> p (c t)"), in_=pr[:, :], func=AF.Exp),
                         [prb], [PT_b[pk][par]])
                for par in range(2):
                    ov = oacc[par][:, 0:260].rearrange("p (c d) -> p c d", c=4)
                    last_blk = (lbk == nblk - 1)
                    for c in range(4):
                        h = 2 * c + par
                        P.op("pe", lambda c=c, h=h, ov=ov, par=par: nc.tensor.matmul(ov[:, c, :], PT[pk][:, par, c, :], Vs[sl][:, j, h * 65:(h + 1) * 65],
                                                                                    start=(first_pv[par] and c == 0), stop=(last_blk and c == 3)),
                             [PT_b[pk][par], Vs_b[sl]], [oacc_b[par]], inc=(c == 3))
                    first_pv[par] = False
        for par in range(2):
            ov = oacc[par][:, 0:260].rearrange("p (c d) -> p c d", c=4)
            P.op("dve", lambda ov=ov, par=par: nc.vector.reciprocal(rec[:, par * 4:(par + 1) * 4], ov[:, :, 64]), [oacc_b[par]], [rec_b])
            for c in range(4):
                h = 2 * c + par
                P.op("dve", lambda ov=ov, c=c, h=h, par=par: nc.vector.tensor_scalar(mix[:, h * 64:(h + 1) * 64], ov[:, c, 0:64],
                                                                                   rec[:, par * 4 + c:par * 4 + c + 1], None, ALU.mult),
                     [oacc_b[par], rec_b], [mix_b])
        P.dma("sp", MIX_d[rows, :], mix[:], [mix_b], [MIX_b[i]], mix_b)
        if debug and i == NP - 1:
            dbg_dump(score[:, 0:min(nk, 1024)], score_b, min(nk, 1024))
            dbg_dump(col(0, 64), bb, 64)

    P.barrier()
    es1.close()
    es2 = ExitStack()
    def sb2(name, shape, dt):
        return es2.enter_context(nc.sbuf_tensor("s2_" + name, list(shape), dt))
    Wg = sb2("Wg", [128, 8, DFF], BF16); Wg_b = P.buf("Wg")
    Wu = sb2("Wu", [128, 8, DFF], BF16); Wu_b = P.buf("Wu")
    Wd = sb2("Wd", [128, NFC, D], BF16); Wd_b = P.buf("Wd")
    Wo = sb2("Wo", [128, 8, D], BF16); Wo_b = P.buf("Wo")
    stage2 = [sb2(f"stage2_{k}", [128, 8, 128], F32) for k in range(2)]
    stage2_b = [P.buf(f"stage2_{k}") for k in range(2)]
    gfin = sb2("gfin", [128, D], F32)
    gffn = sb2("gffn", [128, 8], F32)
    c3_b = P.buf("consts3")
    P.dma("sp", gfin[:], gfin_d[:, :], [], [c3_b], c3_b)
    P.dma("sp", gffn[:], gffn_d[:, :], [], [c3_b], c3_b)
    P.dma("pool", Wo[:], wo_d.rearrange("(c p) n -> p c n", p=128), [], [Wo_b], Wo_b)
    wd_v = wd_d.rearrange("(f p) n -> p f n", p=128)
    for f0 in range(0, NFC, 6):
        f1 = min(NFC, f0 + 6)
        P.dma("pool", Wd[:, f0:f1, :], wd_v[:, f0:f1, :], [], [Wd_b], Wd_b)
    load_weight_folded(Wg, Wg_b, wg_d, DFF, gffn, c3_b, stage2, stage2_b, 128)
    load_weight_folded(Wu, Wu_b, wu_d, DFF, gffn, c3_b, stage2, stage2_b, 128)

    NTOK = GB * 128
    x2t = [sb2(f"x2t{k}", [128, D], F32) for k in range(GB)]; x2t_b = [P.buf(f"x2t{k}") for k in range(GB)]
    mxt = sb2("mxt", [128, D], BF16); mxt_b = P.buf("mxt")
    mxT = sb2("mxT", [128, 8, 128], BF16); mxT_b = P.buf("mxT")
    x1 = [sb2(f"x1_{k}", [128, D], F32) for k in range(GB)]; x1_b = [P.buf(f"x1_{k}") for k in range(GB)]
    h2 = sb2("h2", [128, D], BF16); h2_b = P.buf("h2")
    h2T = sb2("h2T", [128, 8, NTOK], BF16); h2T_b = P.buf("h2T")
    uT = sb2("uT", [128, NFC, NTOK], BF16); uT_b = P.buf("uT")
    sg = [sb2(f"sg{k}", [128, NTOK], BF16) for k in range(2)]; sg_b = [P.buf(f"sg{k}") for k in range(2)]
    junk2, junk2_b = h2, h2_b
    xf = sb2("xf", [128, D], F32); xf_b = P.buf("xf")
    c2 = sb2("c2", [128, 16], F32)
    c2b = {k: P.buf("c2_" + k) for k in ("ssq", "tmp", "rs", "ssq2", "tmp2", "rs2")}

    for g in range(NP // GB):
        for k in range(GB):
            blk = g * GB + k
            rows = slice(blk * 128, (blk + 1) * 128)
            P.dma("sp", x2t[k][:], xo_d[rows, :], [], [x2t_b[k]], x2t_b[k])
            P.dma("sp", mxt[:], MIX_d[rows, :], [MIX_b[blk]], [mxt_b], mxt_b)
            for c in range(8):
                P.op("pe", lambda c=c: nc.tensor.transpose(tp[:, c * 128:(c + 1) * 128], mxt[:, c * 128:(c + 1) * 128], identb[:]),
                     [mxt_b, c_b], [tp_b], inc=(c == 7))
            P.op("dve", lambda: nc.vector.tensor_copy(mxT[:], tp[:, :].rearrange("p (c t) -> p c t", c=8)), [tp_b], [mxT_b])
            for n in range(2):
                pt, pb = next_mm()
                for c in range(8):
                    P.op("pe", lambda c=c, n=n, pt=pt: nc.tensor.matmul(pt[:, :], mxT[:, c, :], Wo[:, c, n * 512:(n + 1) * 512], start=(c == 0), stop=(c == 7)),
                         [mxT_b, Wo_b], [pb], inc=(c == 7))
                P.op("dve", lambda n=n, pt=pt, k=k: nc.vector.tensor_tensor(x1[k][:, n * 512:(n + 1) * 512], pt[:, :], x2t[k][:, n * 512:(n + 1) * 512], ALU.add),
                     [pb, x2t_b[k]], [x1_b[k]])
            P.op("act", lambda k=k: nc.scalar.activation(out=junk2[:], in_=x1[k][:], func=AF.Square, accum_out=c2[:, 0:1]), [x1_b[k]], [junk2_b, c2b["ssq"]])
            rstd_from_ssq(c2[:, 0:1], c2b["ssq"], c2[:, 2:3], c2b["rs"], D, 1, c2[:, 1:2], c2b["tmp"])
            P.op("dve", lambda k=k: nc.vector.tensor_scalar(h2[:], x1[k][:], c2[:, 2:3], None, ALU.mult), [x1_b[k], c2b["rs"]], [h2_b])
            for c in range(8):
                P.op("pe", lambda c=c: nc.tensor.transpose(tp[:, c * 128:(c + 1) * 128], h2[:, c * 128:(c + 1) * 128], identb[:]),
                     [h2_b, c_b], [tp_b], inc=(c == 7))
            P.op("act", lambda k=k: nc.scalar.copy(h2T[:, :, k * 128:(k + 1) * 128], tp[:, :].rearrange("p (c t) -> p c t", c=8)), [tp_b], [h2T_b])
        for f in range(NFC):
            pg, pgb = next_mm()
            for c in range(8):
                P.op("pe", lambda c=c, f=f, pg=pg: nc.tensor.matmul(pg[:, 0:NTOK], Wg[:, c, f * 128:(f + 1) * 128], h2T[:, c, :], start=(c == 0), stop=(c == 7)),
                     [Wg_b, h2T_b], [pgb], inc=(c == 7))
            pu, pub = next_mm()
            for c in range(8):
                P.op("pe", lambda c=c, f=f, pu=pu: nc.tensor.matmul(pu[:, 0:NTOK], Wu[:, c, f * 128:(f + 1) * 128], h2T[:, c, :], start=(c == 0), stop=(c == 7)),
                     [Wu_b, h2T_b], [pub], inc=(c == 7))
            P.op("act", lambda f=f, pg=pg: nc.scalar.activation(out=sg[f % 2][:], in_=pg[:, 0:NTOK], func=AF.Silu), [pgb], [sg_b[f % 2]])
            P.op("dve", lambda f=f, pu=pu: nc.vector.tensor_tensor(uT[:, f, :], pu[:, 0:NTOK], sg[f % 2][:], ALU.mult), [pub, sg_b[f % 2]], [uT_b])
        for k in range(GB):
            blk = g * GB + k
            rows = slice(blk * 128, (blk + 1) * 128)
            for n in range(2):
                pt, pb = next_mm()
                for f in range(NFC):
                    P.op("pe", lambda f=f, n=n, pt=pt, k=k: nc.tensor.matmul(pt[:, :], uT[:, f, k * 128:(k + 1) * 128], Wd[:, f, n * 512:(n + 1) * 512],
                                                                             start=(f == 0), stop=(f == NFC - 1)),
                         [uT_b, Wd_b], [pb], inc=(f == NFC - 1))
                P.op("dve", lambda n=n, pt=pt, k=k: nc.vector.tensor_tensor(xf[:, n * 512:(n + 1) * 512], pt[:, :], x1[k][:, n * 512:(n + 1) * 512], ALU.add),
                     [pb, x1_b[k]], [xf_b])
            P.op("act", lambda: nc.scalar.activation(out=junk2[:], in_=xf[:], func=AF.Square, accum_out=c2[:, 4:5]), [xf_b], [junk2_b, c2b["ssq2"]])
            rstd_from_ssq(c2[:, 4:5], c2b["ssq2"], c2[:, 6:7], c2b["rs2"], D, 1, c2[:, 5:6], c2b["tmp2"])
            o_t, o_b = x2t[k], x2t_b[k]
            P.op("dve", lambda o_t=o_t: nc.vector.scalar_tensor_tensor(out=o_t[:], in0=xf[:], scalar=c2[:, 6:7], in1=gfin[:], op0=ALU.mult, op1=ALU.mult),
                 [xf_b, c2b["rs2"], c3_b], [o_b])
            P.dma("sp", out_d[rows, :], o_t[:], [o_b], [], o_b)
    P.barrier()
    es2.close()
    esp.close()
    es0.close()
    build.stats = dict(ninst=P.ninst, nwait=P.nwait, nsem=P.nsem)
    return nc


def _rel_bucket(n):
    n = np.maximum(n, 0).astype(np.int32)
    max_exact = 16
    nf = np.maximum(n, max_exact).astype(np.float32)
    large = max_exact + (np.log(nf / np.float32(max_exact)) / np.float32(math.log(128 / 16)) * np.float32(16)).astype(np.int32)
    large = np.minimum(large, 31)
    return np.where(n < max_exact, n, large)


def prep_inputs(inputs, NP):
    SEQ = 256 * NP
    x = np.asarray(inputs["x"], dtype=np.float32)
    B = x.shape[0]
    assert x.shape[1] == SEQ
    w_in = np.asarray(inputs["w_in"], dtype=np.float32)[0]
    o = np.cumsum([0, 512, 512, 512, 512, 64, 8, 512, 512, 512, 512])
    qa, ka, va, qi, ki, wi, qb, kb, vb, gb = [w_in[:, o[k]:o[k + 1]] for k in range(10)]
    wk = np.ascontiguousarray(np.concatenate([ka, va, kb, vb, ki], axis=1))
    wq = np.ascontiguousarray(np.concatenate([qa, qi, qb, gb, wi], axis=1))
    f32 = lambda a: np.ascontiguousarray(np.asarray(a, dtype=np.float32))
    col8 = lambda g: f32(np.asarray(g, dtype=np.float32).reshape(8, 128).T)
    bc = lambda v: f32(np.broadcast_to(np.asarray(v, dtype=np.float32).reshape(1, -1), (128, np.asarray(v).size)))
    rel_bias = np.asarray(inputs["rel_bias"], dtype=np.float32)
    kk = np.arange(128)[:, None]
    qq = np.arange(128)[None, :]
    bkt_own = _rel_bucket(qq - kk)
    bkt_oth = _rel_bucket(qq + 128 - kk)
    bown = np.where((qq >= kk)[:, None, :], rel_bias[bkt_own].transpose(0, 2, 1), 0.0)
    both = rel_bias[bkt_oth].transpose(0, 2, 1)
    theta = (1.0 / (np.float32(10000.0) ** np.linspace(0.0, 1.0, 64, dtype=np.float32))).astype(np.float32)
    pos = np.arange(SEQ, dtype=np.float32)
    ang = (pos[:, None] * theta[None, :]).astype(np.float32)
    cos_all = np.cos(ang.astype(np.float64)).astype(np.float32)
    sin_all = np.sin(ang.astype(np.float64)).astype(np.float32)
    gamma = 1.0 - 2.0 ** (-5.0 - np.arange(4, dtype=np.float64))
    ii = np.arange(128, dtype=np.float64)[:, None]
    decq = (gamma[None, :] ** (ii + 1.0)).astype(np.float32)
    deck = ((gamma[None, :] ** (-(ii + 1.0))) * (128.0 ** -0.5)).astype(np.float32)
    gcv = np.broadcast_to((gamma ** 128.0).astype(np.float32)[None, :], (128, 4))
    tri = np.where(np.arange(128)[None, :] <= np.arange(128)[:, None], 0.0, NEG).astype(np.float32)
    trir = (np.arange(128)[None, :] >= np.arange(128)[:, None]).astype(np.float32)
    shared = dict(
        wk=wk, wq=wq, wo=f32(inputs["w_out"][0]), wg=f32(inputs["w_gate"][0]), wu=f32(inputs["w_up"][0]), wd=f32(inputs["w_down"][0]),
        gmix=col8(inputs["norm_mix_g"][0]), gffn=col8(inputs["norm_ffn_g"][0]), gfin=bc(inputs["norm_final_g"]),
        lng=bc(inputs["idx_k_ln_g"][0]), lnb=bc(inputs["idx_k_ln_b"][0]), gn=bc(inputs["ret_gn_g"][0]), b31=bc(rel_bias[31]),
        bown=f32(bown), both=f32(both), decq=f32(decq), deck=f32(deck), gc=f32(gcv),
        ident=np.eye(128, dtype=np.float32), tri=tri, trir=f32(trir),
    )
    in_maps = []
    for core in range(2 * B):
        b, p = core // 2, core % 2
        xb = x[b].reshape(2 * NP, 128, D)
        own_ids = [2 * i + p for i in range(NP)]
        oth_ids = [2 * i - 1 + p for i in range(NP)]
        xo = f32(xb[own_ids].reshape(NP * 128, D))
        xt = np.zeros((NP, 128, D), np.float32)
        cost = np.zeros((NP, 128, 64), np.float32)
        sint = np.zeros((NP, 128, 64), np.float32)
        cb = cos_all.reshape(2 * NP, 128, 64)
        sbk = sin_all.reshape(2 * NP, 128, 64)
        for i, gidx in enumerate(oth_ids):
            if gidx >= 0:
                xt[i] = xb[gidx]
                cost[i] = cb[gidx]
                sint[i] = sbk[gidx]
        kbias = np.zeros((128, 128), np.float32) if p == 1 else np.full((128, 128), NEG, np.float32)
        m = dict(shared)
        m.update(xo=xo, xt=f32(xt.reshape(NP * 128, D)), coso=f32(cb[own_ids].reshape(NP * 128, 64)), sino=f32(sbk[own_ids].reshape(NP * 128, 64)),
                 cost=f32(cost.reshape(NP * 128, 64)), sint=f32(sint.reshape(NP * 128, 64)), kbias=kbias)
        in_maps.append(m)
    return in_maps


def assemble(results, B, NP):
    out = np.zeros((B, 2 * NP, 128, D), np.float32)
    for core in range(2 * B):
        b, p = core // 2, core % 2
        r = np.asarray(results[core]["out"]).reshape(NP, 128, D)
        for i in range(NP):
            out[b, 2 * i + p] = r[i]
    return out.reshape(B, 2 * NP * 128, D)


def kernel(x, norm_mix_g, w_in, idx_k_ln_g, idx_k_ln_b, rel_bias, ret_gn_g, w_out, norm_ffn_g, w_gate, w_up, w_down, norm_final_g):
    inputs = dict(x=x, norm_mix_g=norm_mix_g, w_in=w_in, idx_k_ln_g=idx_k_ln_g, idx_k_ln_b=idx_k_ln_b, rel_bias=rel_bias,
                  ret_gn_g=ret_gn_g, w_out=w_out, norm_ffn_g=norm_ffn_g, w_gate=w_gate, w_up=w_up, w_down=w_down,
                  norm_final_g=norm_final_g)
    inputs = {k: np.asarray(v) for k, v in inputs.items()}
    B, SEQ = inputs["x"].shape[0], inputs["x"].shape[1]
    NP = SEQ // 256
    TOPK = min(256, SEQ // 4)
    nc = build(NP, TOPK)
    in_maps = prep_inputs(inputs, NP)
    res = run_bass_kernel_spmd(nc, in_maps, core_ids=list(range(2 * B)))
    return assemble(res.results, B, NP)
```

```python
import math
from contextlib import ExitStack

import numpy as np
import concourse.bass as bass
import concourse.mybir as mybir
from concourse.bass_utils import run_bass_kernel_spmd

F32 = mybir.dt.float32
BF16 = mybir.dt.bfloat16
U8 = mybir.dt.uint8
ALU = mybir.AluOpType
AF = mybir.ActivationFunctionType
AX = mybir.AxisListType

D = 1024
DFF = 2816
NFC = DFF // 128
EPS = 1e-6
NEG = -1.0e30
MNEG = -30000.0
R_RANGE = 16.0
N_ITER = 18
GB = 2


class Buf:
    __slots__ = ("name", "w", "r", "dsem", "dcnt")

    def __init__(self, name):
        self.name = name
        self.w = None
        self.r = {}
        self.dsem = None
        self.dcnt = 0


class Eng:
    def __init__(self, name, obj):
        self.name = name
        self.obj = obj
        self.sem = None
        self.cnt = 0
        self.seen = {}


class Prog:
    EPOCH = 30000

    def __init__(self, nc, es):
        self.nc = nc
        self.es = es
        self.eng = {
            "pe": Eng("pe", nc.tensor),
            "act": Eng("act", nc.scalar),
            "dve": Eng("dve", nc.vector),
            "pool": Eng("pool", nc.gpsimd),
            "sp": Eng("sp", nc.sync),
        }
        self.nsem = 0
        self.ninst = 0
        self.nwait = 0
        self.allbufs = []
        self.oldsems = []
        for e in self.eng.values():
            e.sem = self.new_sem(e.name)

    def new_sem(self, name):
        self.nsem += 1
        return self.es.enter_context(self.nc.semaphore(f"s{self.nsem}_{name}"))

    def buf(self, name):
        b = Buf(name)
        self.allbufs.append(b)
        return b

    def _need(self, E, t, waits):
        if t is None:
            return
        sem, val = t
        k = id(sem)
        if E.seen.get(k, 0) >= val:
            return
        if E.name == "pe" and sem is E.sem:
            return
        if k in waits:
            if waits[k][1] < val:
                waits[k] = (sem, val)
        else:
            waits[k] = (sem, val)

    def _flush(self, E, waits):
        for k, (sem, val) in waits.items():
            for e2 in self.eng.values():
                if e2.sem is sem and val > e2.cnt:
                    raise RuntimeError(f"wait on pending ticket of {e2.name}: {val}>{e2.cnt} (on {E.name})")
            E.obj.wait_ge(sem, val)
            E.seen[k] = val
            self.nwait += 1

    def _emit_waits(self, E, reads, writes):
        waits = {}
        for b in reads:
            self._need(E, b.w, waits)
        for b in writes:
            self._need(E, b.w, waits)
            for t in b.r.values():
                self._need(E, t, waits)
        self._flush(E, waits)

    def _record(self, t, reads, writes):
        sem, val = t
        k = id(sem)
        for b in reads:
            old = b.r.get(k)
            if old is None or old[1] < val:
                b.r[k] = t
        for b in writes:
            b.w = t
            b.r = {}

    def op(self, en, fn, reads=(), writes=(), inc=True):
        E = self.eng[en]
        self._emit_waits(E, reads, writes)
        ins = fn()
        self.ninst += 1
        if inc:
            if E.cnt >= self.EPOCH:
                self.oldsems.append((E.sem, E.cnt))
                E.sem = self.new_sem(E.name)
                E.cnt = 0
            E.cnt += 1
            ins.then_inc(E.sem, 1)
            t = (E.sem, E.cnt)
        else:
            if E.cnt + 1 > self.EPOCH:
                raise RuntimeError("pending across epoch")
            t = (E.sem, E.cnt + 1)
        self._record(t, reads, writes)
        return ins

    def dma(self, en, out, in_, reads, writes, sembuf):
        E = self.eng[en]
        self._emit_waits(E, reads, writes)
        if sembuf.dsem is None:
            sembuf.dsem = self.new_sem("d_" + sembuf.name)
        ins = E.obj.dma_start(out=out, in_=in_)
        sembuf.dcnt += 16
        ins.then_inc(sembuf.dsem, 16)
        self.ninst += 1
        self._record((sembuf.dsem, sembuf.dcnt), reads, writes)
        return ins

    def barrier(self):
        for E in self.eng.values():
            waits = {}
            for e2 in self.eng.values():
                if e2 is not E and e2.cnt > 0:
                    self._need(E, (e2.sem, e2.cnt), waits)
            for b in self.allbufs:
                if b.dsem is not None and b.dcnt > 0:
                    self._need(E, (b.dsem, b.dcnt), waits)
            self._flush(E, waits)


def build(NP, TOPK, debug=False):
    L = 256 * NP
    NT = 128 * NP
    nc = bass.Bass("TRN2", target_bir_lowering=False)
    es0 = ExitStack()
    P = Prog(nc, es0)

    def din(name, shape, dt=F32):
        return nc.dram_tensor(name, list(shape), dt, kind="ExternalInput").ap()

    xo_d = din("xo", [NT, D])
    xt_d = din("xt", [NT, D])
    wk_d = din("wk", [D, 2112])
    wq_d = din("wq", [D, 2056])
    wo_d = din("wo", [D, D])
    wg_d = din("wg", [D, DFF])
    wu_d = din("wu", [D, DFF])
    wd_d = din("wd", [DFF, D])
    gmix_d = din("gmix", [128, 8])
    gffn_d = din("gffn", [128, 8])
    gfin_d = din("gfin", [128, D])
    lng_d = din("lng", [128, 64])
    lnb_d = din("lnb", [128, 64])
    gn_d = din("gn", [128, 512])
    b31_d = din("b31", [128, 8])
    bown_d = din("bown", [128, 8, 128])
    both_d = din("both", [128, 8, 128])
    coso_d = din("coso", [NT, 64])
    sino_d = din("sino", [NT, 64])
    cost_d = din("cost", [NT, 64])
    sint_d = din("sint", [NT, 64])
    decq_d = din("decq", [128, 4])
    deck_d = din("deck", [128, 4])
    gc_d = din("gc", [128, 4])
    ident_d = din("ident", [128, 128])
    tri_d = din("tri", [128, 128])
    kbias_d = din("kbias", [128, 128])
    trir_d = din("trir", [128, 128])
    out_d = nc.dram_tensor("out", [NT, D], F32, kind="ExternalOutput").ap()
    dbg_d = nc.dram_tensor("dbg", [128, 8192], F32, kind="ExternalOutput").ap() if debug else None
    KT_d = nc.dram_tensor("KTs", [128, 4, L], BF16, kind="Internal").ap()
    V_d = nc.dram_tensor("Vs", [2 * NP, 128, 520], BF16, kind="Internal").ap()
    MIX_d = nc.dram_tensor("MIXs", [NT, D], BF16, kind="Internal").ap()
    KT_b = [P.buf(f"KTd{j}") for j in range(2 * NP)]
    V_b = [P.buf(f"Vd{j}") for j in range(2 * NP)]
    MIX_b = [P.buf(f"MIXd{j}") for j in range(NP)]

    esp = ExitStack()
    def ps(name, shape, dt):
        return esp.enter_context(nc.psum_tensor("p_" + name, list(shape), dt))
    mm = [ps(f"mm{k}", [128, 512], F32) for k in range(2)]
    mm_b = [P.buf(f"mm{k}") for k in range(2)]
    tp = ps("tp", [128, 1024], BF16)
    tp_b = P.buf("tp")
    acc = [ps(f"acc{k}", [128, 512], F32) for k in range(2)]
    acc_b = [P.buf(f"acc{k}") for k in range(2)]
    rot = [ps(f"rot{k}", [128, 512], F32) for k in range(3)]
    rot_b = [P.buf(f"rot{k}") for k in range(3)]
    cnt = {"mm": 0, "rot": 0}

    def next_mm():
        k = cnt["mm"] % 2
        cnt["mm"] += 1
        return mm[k], mm_b[k]

    def next_rot():
        k = cnt["rot"] % 3
        cnt["rot"] += 1
        return rot[k], rot_b[k]

    def sb0(name, shape, dt):
        return es0.enter_context(nc.sbuf_tensor("s0_" + name, list(shape), dt))
    identf = sb0("identf", [128, 128], F32)
    identb = sb0("identb", [128, 128], BF16)
    mhalf = sb0("mhalf", [128, 8], F32)
    c_b = P.buf("consts")
    P.dma("sp", identf[:], ident_d[:, :], [], [c_b], c_b)
    P.op("pool", lambda: nc.gpsimd.memset(mhalf[:], -0.5), [], [c_b])
    P.op("pool", lambda: nc.gpsimd.tensor_copy(identb[:], identf[:]), [c_b], [c_b])

    dbg_state = {"off": 0}

    def rstd_from_ssq(ssq, ssq_b, rs, rs_b, n, width, tmp, tmp_b):
        P.op("dve", lambda: nc.vector.tensor_scalar(tmp, ssq, 1.0 / n, EPS, ALU.mult, ALU.add), [ssq_b], [tmp_b])
        P.op("pool", lambda: nc.gpsimd.tensor_tensor(rs, tmp, mhalf[:, 0:width], ALU.pow), [tmp_b, c_b], [rs_b])

    def load_weight_folded(dst, dst_b, src_d, ncols, gcol, gcol_b, stage, stage_b, CH):
        src_v = src_d.rearrange("(c p) n -> p c n", p=128)
        k = 0
        for n0 in range(0, ncols, CH):
            w = min(CH, ncols - n0)
            st, st_b = stage[k % 2], stage_b[k % 2]
            P.dma("sp", st[:, :, 0:w], src_v[:, :, n0:n0 + w], [], [st_b], st_b)
            for c in range(8):
                e = ("dve", "pool", "act")[(k * 8 + c) % 3]
                if e == "dve":
                    P.op("dve", lambda c=c: nc.vector.tensor_scalar(dst[:, c, n0:n0 + w], st[:, c, 0:w], gcol[:, c:c + 1], None, ALU.mult),
                         [st_b, gcol_b], [dst_b])
                elif e == "pool":
                    P.op("pool", lambda c=c: nc.gpsimd.tensor_scalar(dst[:, c, n0:n0 + w], st[:, c, 0:w], gcol[:, c:c + 1], 1.0, ALU.mult, ALU.mult),
                         [st_b, gcol_b], [dst_b])
                else:
                    P.op("act", lambda c=c: nc.scalar.activation(out=dst[:, c, n0:n0 + w], in_=st[:, c, 0:w], func=AF.Copy, scale=gcol[:, c:c + 1]),
                         [st_b, gcol_b], [dst_b])
            k += 1

    def norm_transpose(x_t, x_b, xb_t, xb_b, hT, hT_b, ssq, ssq_b, junk, junk_b, col_off):
        P.op("act", lambda: nc.scalar.activation(out=junk, in_=x_t, func=AF.Square, accum_out=ssq), [x_b], [junk_b, ssq_b])
        P.op("pool", lambda: nc.gpsimd.tensor_copy(xb_t, x_t), [x_b], [xb_b])
        for c in range(8):
            P.op("pe", lambda c=c: nc.tensor.transpose(tp[:, c * 128:(c + 1) * 128], xb_t[:, c * 128:(c + 1) * 128], identb[:]),
                 [xb_b, c_b], [tp_b], inc=(c == 7))
        P.op("dve", lambda: nc.vector.tensor_copy(hT[:, :, col_off:col_off + 128], tp[:, :].rearrange("p (c t) -> p c t", c=8)),
             [tp_b], [hT_b])

    assert NP % 4 == 0
    es1 = ExitStack()
    def sb(name, shape, dt):
        return es1.enter_context(nc.sbuf_tensor("s1_" + name, list(shape), dt))

    Wk = sb("Wk", [128, 8, 2112], BF16); Wk_b = P.buf("Wk")
    Wq = sb("Wq", [128, 8, 2056], BF16); Wq_b = P.buf("Wq")
    kidxT = sb("kidxT", [128, L // 2], BF16); kidxT_b = [P.buf(f"kidxT{j}") for j in range(2 * NP)]
    score = sb("score", [128, L], F32); score_b = P.buf("score")
    msk = sb("msk", [128, L], U8); msk_b = P.buf("msk")
    gmix = sb("gmix", [128, 8], F32)
    lng = sb("lng", [128, 64], F32); lnb = sb("lnb", [128, 64], F32)
    gn = sb("gn", [128, 512], F32)
    b31 = sb("b31", [128, 8], F32)
    bown = sb("bown", [128, 8, 128], BF16); both = sb("both", [128, 8, 128], BF16)
    decq = sb("decq", [128, 4], F32); deck = sb("deck", [128, 4], F32); gc = sb("gc", [128, 4], F32)
    tri = sb("tri", [128, 128], F32); kbias = sb("kbias", [128, 128], F32); trir = sb("trir", [128, 128], F32)
    k1_b = P.buf("consts1")
    for t_, d_ in ((gmix, gmix_d), (lng, lng_d), (lnb, lnb_d), (gn, gn_d), (b31, b31_d), (decq, decq_d),
                   (deck, deck_d), (gc, gc_d), (tri, tri_d), (kbias, kbias_d), (trir, trir_d)):
        P.dma("sp", t_[:], d_[:, :], [], [k1_b], k1_b)
    es_st = ExitStack()
    stage = [es_st.enter_context(nc.sbuf_tensor(f"s1_stage{k}", [128, 8, 256], F32)) for k in range(2)]
    stage_b = [P.buf(f"stage{k}") for k in range(2)]
    btmp, bt_b = stage[0], stage_b[0]
    for tab, tab_d in ((bown, bown_d), (both, both_d)):
        P.dma("sp", btmp[:, :, 0:128], tab_d[:, :, :], [], [bt_b], bt_b)
        for h in range(8):
            P.op("dve", lambda h=h, tab=tab: nc.vector.tensor_scalar(tab[:, h, :], btmp[:, h, 0:128], b31[:, h:h + 1], None, ALU.subtract),
                 [bt_b, k1_b], [k1_b])
    load_weight_folded(Wk, Wk_b, wk_d, 2112, gmix, k1_b, stage, stage_b, 256)
    load_weight_folded(Wq, Wq_b, wq_d, 2056, gmix, k1_b, stage, stage_b, 256)
    P.barrier()
    es_st.close()

    xo = sb("xo", [128, D], F32); xo_b = P.buf("xo")
    xt = sb("xt", [128, D], F32); xt_b = P.buf("xt")
    xbf = sb("xbf", [128, D], BF16); xbf_b = P.buf("xbf")
    hTo = sb("hTo", [128, 8, 128], BF16); hTo_b = P.buf("hTo")
    hTt, hTt_b = hTo, hTo_b
    cols = sb("cols", [128, 64], F32)
    colb = {k: P.buf("col_" + k) for k in ("ssq_o", "ssq_t", "rs_o", "rs_t", "tmp", "rsq8", "rsdq", "rsdk_o", "rsdk_t",
                                            "ln", "w", "ret", "tr", "cn", "sg", "tt", "gg", "thr0", "thr1")}
    C_SSQ_O, C_SSQ_T, C_RS_O, C_RS_T, C_TMP, C_RSQ8 = 0, 1, 2, 3, 4, 5
    C_RSDQ, C_RSDK_O, C_RSDK_T = 8, 12, 16
    C_LN = 20
    C_W = 28
    C_RET = 36
    C_TR, C_CN, C_SG, C_TT, C_GG = 48, 49, 50, 51, 52
    C_THR = 54
    cos_o = sb("cos_o", [128, 64], F32); sin_o = sb("sin_o", [128, 64], F32)
    cos_t = sb("cos_t", [128, 64], F32); sin_t = sb("sin_t", [128, 64], F32)
    tab_b = P.buf("rot_tabs")
    ka_tok = sb("ka_tok", [128, 512], BF16); ka_tok_b = P.buf("ka_tok")
    kaT_blk = [sb(f"kaT_blk{k}", [128, 4, 128], BF16) for k in range(2)]; kaT_blk_b = [P.buf(f"kaT_blk{k}") for k in range(2)]
    va_tok = [sb(f"va_tok{k}", [128, 8, 65], BF16) for k in range(2)]; va_tok_b = [P.buf(f"va_tok{k}") for k in range(2)]
    kx = sb("kx", [128, 64], F32); kx_b = P.buf("kx")
    kc = sb("kc", [128, 64], F32); kc_b = P.buf("kc")
    kn = sb("kn", [128, 2, 64], BF16); kn_b = P.buf("kn")
    ftmp = sb("ftmp", [128, 512], F32); ftmp_b = P.buf("ftmp")
    rt1 = sb("rt1", [128, 4, 64], F32); rt2 = sb("rt2", [128, 4, 64], F32); rt1_b = P.buf("rt1"); rt2_b = P.buf("rt2")
    Kp_t = sb("Kp_t", [128, 512], BF16); Kp_t_b = P.buf("Kp_t")
    Kp_o = sb("Kp_o", [128, 512], BF16); Kp_o_b = P.buf("Kp_o")
    KpT = sb("KpT", [128, 4, 128], BF16); KpT_b = P.buf("KpT")
    Vb_t = sb("Vb_t", [128, 512], BF16); Vb_t_b = P.buf("Vb_t")
    Vb_o = sb("Vb_o", [128, 512], BF16); Vb_o_b = P.buf("Vb_o")
    qa_tok = sb("qa_tok", [128, 512], BF16); qa_tok_b = P.buf("qa_tok")
    qaT = [sb(f"qaT{k}", [128, 4, 128], BF16) for k in range(3)]; qaT_b = [P.buf(f"qaT{k}") for k in range(3)]
    qi_tok = sb("qi_tok", [128, 8, 2, 64], BF16); qi_tok_b = P.buf("qi_tok")
    qiT = [sb(f"qiT{k}", [128, 8, 128], BF16) for k in range(2)]; qiT_b = [P.buf(f"qiT{k}") for k in range(2)]
    Qp = sb("Qp", [128, 512], BF16); Qp_b = P.buf("Qp")
    QpT = sb("QpT", [128, 4, 128], BF16); QpT_b = P.buf("QpT")
    gsil = sb("gsil", [128, 512], BF16); gsil_b = P.buf("gsil")
    diag = [sb(f"diag{k}", [128, 8, 128], BF16) for k in range(2)]; diag_b = [P.buf(f"diag{k}") for k in range(2)]
    S = sb("S", [128, 4, 128], F32); S_b = P.buf("S")
    Sb = sb("Sb", [128, 4, 128], BF16); Sb_b = P.buf("Sb")
    inTD = sb("inTD", [128, 4, 128], BF16); inTD_b = P.buf("inTD")
    ytmp, ytmp_b = ftmp, ftmp_b
    stmp, stmp_b = ftmp[:, :].rearrange("p (h e) -> p h e", h=4), ftmp_b
    mix = [sb(f"mix{k}", [128, D], BF16) for k in range(3)]; mix_b = [P.buf(f"mix{k}") for k in range(3)]
    NRH = 4
    Rh = [sb(f"Rh{h}", [128, 512], BF16) for h in range(NRH)]; Rh_b = [P.buf(f"Rh{h}") for h in range(NRH)]

    def n_act_of(nk_):
        return ((nk_ * 55 // 100) // 128) * 128
    NACT_MAX = max(1024, n_act_of(L))
    NDVE_MAX = max((2 * i_ + 2) * 128 - n_act_of((2 * i_ + 2) * 128) for i_ in range(NP))
    junk_a = sb("junk_a", [128, NACT_MAX], U8); junk_a_b = P.buf("junk_a")
    junk_d = sb("junk_d", [128, NDVE_MAX], U8); junk_d_b = P.buf("junk_d")
    junkx, junkx_b = junk_a, P.buf("junkx")
    AB = 2
    mk = [sb(f"mk{k}", [128, AB * 128], BF16) for k in range(2)]; mk_b = [P.buf(f"mk{k}") for k in range(2)]
    mkT = [sb(f"mkT{k}", [128, AB, 128], BF16) for k in range(2)]; mkT_b = [P.buf(f"mkT{k}") for k in range(2)]
    KTs = [sb(f"KTs{k}", [128, 4, AB * 128], BF16) for k in range(2)]; KTs_b = [P.buf(f"KTs{k}") for k in range(2)]
    Vs = [sb(f"Vs{k}", [128, AB, 520], BF16) for k in range(2)]; Vs_b = [P.buf(f"Vs{k}") for k in range(2)]
    PT = [sb(f"PT{k}", [128, 2, 4, 128], BF16) for k in range(2)]; PT_b = [[P.buf(f"PT{k}_{r}") for r in range(2)] for k in range(2)]
    rec = sb("rec", [128, 8], F32); rec_b = P.buf("rec")

    mmA, mmA_b = mm[0], mm_b[0]
    accB, accB_b = mm[1], mm_b[1]
    dotsB, dotsB_b = rot[2], rot_b[2]
    rotD = [(rot[0], rot_b[0]), (rot[1], rot_b[1])]
    dcnt = {"k": 0}

    def next_rotD():
        k = dcnt["k"] % 2
        dcnt["k"] += 1
        return rotD[k]

    P.op("dve", lambda: nc.vector.memset(S[:], 0.0), [], [S_b])
    P.op("dve", lambda: nc.vector.memset(Sb[:], 0.0), [], [Sb_b])
    for k in range(2):
        P.op("pool", lambda k=k: nc.gpsimd.memset(va_tok[k][:], 1.0), [], [va_tok_b[k]])

    def col(c, w=1):
        return cols[:, c:c + w]

    def evac_scaled(eng_sel, dst, src, scale_col, scale_b, dst_b, src_b):
        if eng_sel == "act":
            P.op("act", lambda: nc.scalar.activation(out=dst, in_=src, func=AF.Copy, scale=scale_col), [src_b, scale_b], [dst_b])
        else:
            P.op("dve", lambda: nc.vector.tensor_scalar(dst, src, scale_col, None, ALU.mult), [src_b, scale_b], [dst_b])

    def project(W, W_b, c0, ncols, hT, hT_b):
        pt, pb = mmA, mmA_b
        for c in range(8):
            P.op("pe", lambda c=c: nc.tensor.matmul(pt[:, 0:ncols], hT[:, c, :], W[:, c, c0:c0 + ncols], start=(c == 0), stop=(c == 7)),
                 [hT_b, W_b], [pb], inc=(c == 7))
        return pt, pb

    def rotate(dst, dst_b, src3, cos_t_, sin_t_, src_b):
        cb = cos_t_[:, :].unsqueeze(1).to_broadcast([128, 4, 64])
        sbb = sin_t_[:, :].unsqueeze(1).to_broadcast([128, 4, 64])
        x1 = src3[:, :, 0:64]
        x2 = src3[:, :, 64:128]
        P.op("dve", lambda: nc.vector.tensor_tensor(rt1[:], x1, cb, ALU.mult), [src_b, tab_b], [rt1_b])
        P.op("pool", lambda: nc.gpsimd.tensor_tensor(rt2[:], x2, sbb, ALU.mult), [src_b, tab_b], [rt2_b])
        P.op("dve", lambda: nc.vector.tensor_tensor(dst[:, :, 0:64], rt1[:], rt2[:], ALU.subtract), [rt1_b, rt2_b], [dst_b])
        P.op("dve", lambda: nc.vector.tensor_tensor(rt1[:], x2, cb, ALU.mult), [src_b, tab_b], [rt1_b])
        P.op("pool", lambda: nc.gpsimd.tensor_tensor(rt2[:], x1, sbb, ALU.mult), [src_b, tab_b], [rt2_b])
        P.op("dve", lambda: nc.vector.tensor_tensor(dst[:, :, 64:128], rt1[:], rt2[:], ALU.add), [rt1_b, rt2_b], [dst_b])

    def kside(lb, hT, hT_b, rs_c, rs_b, rsdk_c, rsdk_b, cos_t_, sin_t_, Kp, Kp_b, Vb, Vb_b):
        slot = lb % 2
        pt, pb = project(Wk, Wk_b, 0, 512, hT, hT_b)
        evac_scaled("act", ka_tok[:], pt[:, :], col(rs_c), rs_b, ka_tok_b, pb)
        yield 2.3
        for c in range(4):
            P.op("pe", lambda c=c: nc.tensor.transpose(tp[:, c * 128:(c + 1) * 128], ka_tok[:, c * 128:(c + 1) * 128], identb[:]),
                 [ka_tok_b, c_b], [tp_b], inc=(c == 3))
        P.op("dve", lambda: nc.vector.tensor_copy(kaT_blk[slot][:], tp[:, 0:512].rearrange("p (c t) -> p c t", c=4)),
             [tp_b], [kaT_blk_b[slot]])
        P.dma("sp", KT_d[:, :, lb * 128:(lb + 1) * 128], kaT_blk[slot][:], [kaT_blk_b[slot]], [KT_b[lb]], kaT_blk_b[slot])
        yield 0.8
        pt, pb = project(Wk, Wk_b, 512, 512, hT, hT_b)
        evac_scaled("dve", va_tok[slot][:, :, 0:64], pt[:, :].rearrange("p (h d) -> p h d", h=8), col(rs_c), rs_b, va_tok_b[slot], pb)
        P.dma("sp", V_d[lb, :, :], va_tok[slot][:].rearrange("p h d -> p (h d)"), [va_tok_b[slot]], [V_b[lb]], va_tok_b[slot])
        yield 2.3
        pt, pb = project(Wk, Wk_b, 1024, 512, hT, hT_b)
        for h in range(4):
            evac_scaled("act" if h % 2 == 0 else "dve", ftmp[:, h * 128:(h + 1) * 128], pt[:, h * 128:(h + 1) * 128],
                        col(rsdk_c + h), rsdk_b, ftmp_b, pb)
        yield 2.3
        rotate(Kp[:, :].rearrange("p (h d) -> p h d", h=4), Kp_b, ftmp[:, :].rearrange("p (h d) -> p h d", h=4), cos_t_, sin_t_, ftmp_b)
        yield 1.0
        pt, pb = project(Wk, Wk_b, 1536, 512, hT, hT_b)
        evac_scaled("act", Vb[:], pt[:, :], col(rs_c), rs_b, Vb_b, pb)
        yield 2.3
        pt, pb = project(Wk, Wk_b, 2048, 64, hT, hT_b)
        lb_ = colb["ln"]
        P.op("act", lambda: nc.scalar.activation(out=kx[:], in_=pt[:, 0:64], func=AF.Copy, scale=col(rs_c), accum_out=col(C_LN)),
             [pb, rs_b], [kx_b, lb_])
        P.op("dve", lambda: nc.vector.tensor_scalar(col(C_LN + 1), col(C_LN), -1.0 / 64, None, ALU.mult), [lb_], [lb_])
        P.op("act", lambda: nc.scalar.activation(out=kc[:], in_=kx[:], func=AF.Square, bias=col(C_LN + 1), accum_out=col(C_LN + 2)),
             [kx_b, lb_], [kc_b, lb_])
        P.op("dve", lambda: nc.vector.tensor_scalar(col(C_LN + 3), col(C_LN + 2), 1.0 / 64, EPS, ALU.mult, ALU.add), [lb_], [lb_])
        P.op("pool", lambda: nc.gpsimd.tensor_tensor(col(C_LN + 4), col(C_LN + 3), mhalf[:, 0:1], ALU.pow), [lb_, c_b], [lb_])
        yield 1.0
        P.op("dve", lambda: nc.vector.tensor_scalar(kc[:], kx[:], col(C_LN + 1), col(C_LN + 4), ALU.add, ALU.mult), [kx_b, lb_], [kc_b])
        P.op("dve", lambda: nc.vector.tensor_tensor(kc[:], kc[:], lng[:], ALU.mult), [kc_b, k1_b], [kc_b])
        P.op("dve", lambda: nc.vector.tensor_tensor(kn[:, 0, :], kc[:], lnb[:], ALU.add), [kc_b, k1_b], [kn_b])
        P.op("pool", lambda: nc.gpsimd.tensor_copy(kn[:, 1, :], kn[:, 0, :]), [kn_b], [kn_b])
        P.op("pe", lambda: nc.tensor.transpose(tp[:, 0:128], kn[:, :, :].rearrange("p a d -> p (a d)"), identb[:]), [kn_b, c_b], [tp_b])
        hf = lb // NP
        cc = (lb % NP) * 128
        P.op("act", lambda: nc.scalar.copy(kidxT[64 * hf:64 * hf + 64, cc:cc + 128], tp[64 * hf:64 * hf + 64, 0:128]), [tp_b], [kidxT_b[lb]])
        yield 1.0

    def state_update(Kp, Kp_b, Vb, Vb_b):
        pt, pb = mmA, mmA_b
        for h in range(4):
            P.op("pe", lambda h=h: nc.tensor.matmul(pt[:, h * 128:(h + 1) * 128], Kp[:, h * 128:(h + 1) * 128], Vb[:, h * 128:(h + 1) * 128],
                                                    start=(h == 0), stop=(h == 3)), [Kp_b, Vb_b], [pb], inc=(h == 3))
        P.op("dve", lambda: nc.vector.tensor_tensor(stmp, pt[:, :].rearrange("p (h e) -> p h e", h=4), S[:], ALU.add), [pb, S_b], [stmp_b])
        P.op("dve", lambda: nc.vector.tensor_tensor(S[:], stmp, gc[:, :].unsqueeze(2).to_broadcast([128, 4, 128]), ALU.mult),
             [stmp_b, k1_b], [S_b])
        P.op("act", lambda: nc.scalar.copy(Sb[:], S[:]), [S_b], [Sb_b])

    def gen_A(i):
        lo_t, lo_o = 2 * i, 2 * i + 1
        rows = slice(i * 128, (i + 1) * 128)
        s3, s2 = i % 3, i % 2
        P.dma("sp", xt[:], xt_d[rows, :], [], [xt_b], xt_b)
        P.dma("sp", xo[:], xo_d[rows, :], [], [xo_b], xo_b)
        for t_, d_ in ((cos_o, coso_d), (sin_o, sino_d), (cos_t, cost_d), (sin_t, sint_d)):
            P.dma("sp", t_[:], d_[rows, :], [], [tab_b], tab_b)
        norm_transpose(xt[:], xt_b, xbf[:], xbf_b, hTt, hTt_b, col(C_SSQ_T), colb["ssq_t"], junkx[:, 0:D], junkx_b, 0)
        rstd_from_ssq(col(C_SSQ_T), colb["ssq_t"], col(C_RS_T), colb["rs_t"], D, 1, col(C_TMP), colb["tmp"])
        P.op("dve", lambda: nc.vector.tensor_scalar(col(C_RSDK_T, 4), deck[:], col(C_RS_T), None, ALU.mult), [k1_b, colb["rs_t"]], [colb["rsdk_t"]])
        yield 2.5
        yield from kside(lo_t, hTt, hTt_b, C_RS_T, colb["rs_t"], C_RSDK_T, colb["rsdk_t"], cos_t, sin_t, Kp_t, Kp_t_b, Vb_t, Vb_t_b)
        norm_transpose(xo[:], xo_b, xbf[:], xbf_b, hTo, hTo_b, col(C_SSQ_O), colb["ssq_o"], junkx[:, 0:D], junkx_b, 0)
        rstd_from_ssq(col(C_SSQ_O), colb["ssq_o"], col(C_RS_O), colb["rs_o"], D, 1, col(C_TMP), colb["tmp"])
        P.op("dve", lambda: nc.vector.tensor_scalar(col(C_RSDK_O, 4), deck[:], col(C_RS_O), None, ALU.mult), [k1_b, colb["rs_o"]], [colb["rsdk_o"]])
        P.op("dve", lambda: nc.vector.tensor_scalar(col(C_RSDQ, 4), decq[:], col(C_RS_O), None, ALU.mult), [k1_b, colb["rs_o"]], [colb["rsdq"]])
        P.op("dve", lambda: nc.vector.tensor_scalar(col(C_RSQ8), col(C_RS_O), 0.125, None, ALU.mult), [colb["rs_o"]], [colb["rsq8"]])
        yield 2.5
        yield from kside(lo_o, hTo, hTo_b, C_RS_O, colb["rs_o"], C_RSDK_O, colb["rsdk_o"], cos_o, sin_o, Kp_o, Kp_o_b, Vb_o, Vb_o_b)
        for h in range(4):
            P.op("pe", lambda h=h: nc.tensor.transpose(tp[:, h * 128:(h + 1) * 128], Kp_o[:, h * 128:(h + 1) * 128], identb[:]),
                 [Kp_o_b, c_b], [tp_b], inc=(h == 3))
        P.op("act", lambda: nc.scalar.copy(KpT[:], tp[:, 0:512].rearrange("p (h t) -> p h t", h=4)), [tp_b], [KpT_b])
        yield 0.8
        pt, pb = project(Wq, Wq_b, 0, 512, hTo, hTo_b)
        evac_scaled("act", qa_tok[:], pt[:, :], col(C_RSQ8), colb["rsq8"], qa_tok_b, pb)
        yield 2.3
        for c in range(4):
            P.op("pe", lambda c=c: nc.tensor.transpose(tp[:, c * 128:(c + 1) * 128], qa_tok[:, c * 128:(c + 1) * 128], identb[:]),
                 [qa_tok_b, c_b], [tp_b], inc=(c == 3))
        P.op("dve", lambda: nc.vector.tensor_copy(qaT[s3][:], tp[:, 0:512].rearrange("p (c t) -> p c t", c=4)), [tp_b], [qaT_b[s3]])
        yield 0.8
        pt, pb = project(Wq, Wq_b, 512, 512, hTo, hTo_b)
        pv8 = pt[:, :].rearrange("p (h d) -> p h d", h=8)
        evac_scaled("dve", qi_tok[:, :, 0, :], pv8, col(C_RSQ8), colb["rsq8"], qi_tok_b, pb)
        evac_scaled("act", qi_tok[:, :, 1, :], pv8, col(C_RSQ8), colb["rsq8"], qi_tok_b, pb)
        yield 2.5
        for h in range(8):
            P.op("pe", lambda h=h: nc.tensor.transpose(tp[:, h * 128:(h + 1) * 128], qi_tok[:, h, :, :].rearrange("p a d -> p (a d)"), identb[:]),
                 [qi_tok_b, c_b], [tp_b], inc=(h == 7))
        P.op("act", lambda: nc.scalar.copy(qiT[s2][:], tp[:, :].rearrange("p (h t) -> p h t", h=8)), [tp_b], [qiT_b[s2]])
        yield 1.5
        pt, pb = project(Wq, Wq_b, 1024, 512, hTo, hTo_b)
        for h in range(4):
            evac_scaled("act" if h % 2 == 0 else "dve", ftmp[:, h * 128:(h + 1) * 128], pt[:, h * 128:(h + 1) * 128],
                        col(C_RSDQ + h), colb["rsdq"], ftmp_b, pb)
        yield 2.3
        rotate(Qp[:, :].rearrange("p (h d) -> p h d", h=4), Qp_b, ftmp[:, :].rearrange("p (h d) -> p h d", h=4), cos_o, sin_o, ftmp_b)
        for h in range(4):
            P.op("pe", lambda h=h: nc.tensor.transpose(tp[:, h * 128:(h + 1) * 128], Qp[:, h * 128:(h + 1) * 128], identb[:]),
                 [Qp_b, c_b], [tp_b], inc=(h == 3))
        P.op("dve", lambda: nc.vector.tensor_copy(QpT[:], tp[:, 0:512].rearrange("p (h t) -> p h t", h=4)), [tp_b], [QpT_b])
        yield 1.8
        pt, pb = project(Wq, Wq_b, 1536, 512, hTo, hTo_b)
        P.op("act", lambda: nc.scalar.activation(out=gsil[:], in_=pt[:, :], func=AF.Silu, scale=col(C_RS_O)), [pb, colb["rs_o"]], [gsil_b])
        yield 2.3
        pt, pb = project(Wq, Wq_b, 2048, 8, hTo, hTo_b)
        P.op("dve", lambda: nc.vector.tensor_scalar(col(C_W, 8), pt[:, 0:8], col(C_RS_O), 8.0 ** -0.5, ALU.mult, ALU.mult),
             [pb, colb["rs_o"]], [colb["w"]])
        for h in range(8):
            P.op("pool", lambda h=h: nc.gpsimd.tensor_scalar(diag[s2][:, h, :], identf[:], col(C_W + h), 1.0, ALU.mult, ALU.mult),
                 [c_b, colb["w"]], [diag_b[s2]])
        yield 1.0
        state_update(Kp_t, Kp_t_b, Vb_t, Vb_t_b)
        yield 1.5
        pt, pb = mmA, mmA_b
        for h in range(4):
            P.op("pe", lambda h=h: nc.tensor.matmul(pt[:, h * 128:(h + 1) * 128], KpT[:, h, :], QpT[:, h, :], start=(h == 0), stop=(h == 3)),
                 [KpT_b, QpT_b], [pb], inc=(h == 3))
        P.op("dve", lambda: nc.vector.tensor_tensor(inTD[:], pt[:, :].rearrange("p (h t) -> p h t", h=4),
                                                    trir[:, :].unsqueeze(1).to_broadcast([128, 4, 128]), ALU.mult), [pb, k1_b], [inTD_b])
        yield 1.2
        py, pyb = mmA, mmA_b
        for h in range(4):
            P.op("pe", lambda h=h: nc.tensor.matmul(py[:, h * 128:(h + 1) * 128], inTD[:, h, :], Vb_o[:, h * 128:(h + 1) * 128],
                                                    start=(h == 0), stop=False), [inTD_b, Vb_o_b], [pyb], inc=False)
        for h in range(4):
            P.op("pe", lambda h=h: nc.tensor.matmul(py[:, h * 128:(h + 1) * 128], QpT[:, h, :], Sb[:, h, :], start=False, stop=(h == 3)),
                 [QpT_b, Sb_b], [pyb], inc=(h == 3))
        rb_ = colb["ret"]
        for h in range(4):
            P.op("act", lambda h=h: nc.scalar.activation(out=ytmp[:, h * 128:(h + 1) * 128], in_=py[:, h * 128:(h + 1) * 128], func=AF.Square,
                                                         accum_out=col(C_RET + h)), [pyb], [ytmp_b, rb_])
        rstd_from_ssq(col(C_RET, 4), rb_, col(C_RET + 8, 4), rb_, 128, 4, col(C_RET + 4, 4), rb_)
        yield 2.0
        for h in range(4):
            P.op("dve", lambda h=h: nc.vector.scalar_tensor_tensor(out=ytmp[:, h * 128:(h + 1) * 128], in0=py[:, h * 128:(h + 1) * 128],
                                                                   scalar=col(C_RET + 8 + h), in1=gn[:, h * 128:(h + 1) * 128],
                                                                   op0=ALU.mult, op1=ALU.mult), [pyb, rb_, k1_b], [ytmp_b])
        P.op("dve", lambda: nc.vector.tensor_tensor(mix[s3][:, 512:1024], ytmp[:], gsil[:], ALU.mult), [ytmp_b, gsil_b], [mix_b[s3]])
        yield 1.5
        state_update(Kp_o, Kp_o_b, Vb_o, Vb_o_b)
        yield 1.5

    def gen_BC(i):
        s2 = i % 2
        nblk = 2 * i + 2
        nk = nblk * 128
        nslab = (nblk + 3) // 4
        for s in range(nslab):
            b0 = 4 * s
            nb = min(4, nblk - b0)
            ncol = nb * 128
            k0 = b0 * 128
            hf = b0 // NP
            cc = (b0 % NP) * 128
            kb_read = [kidxT_b[b0 + j] for j in range(nb)]
            pend = []
            for h in range(8):
                P.op("pe", lambda h=h: nc.tensor.matmul(dotsB[:, 0:ncol], qiT[s2][64 * hf:64 * hf + 64, h, :],
                                                        kidxT[64 * hf:64 * hf + 64, cc:cc + ncol], start=True, stop=True),
                     [qiT_b[s2]] + kb_read, [dotsB_b])
                r = h % NRH
                if h % 2 == 0:
                    P.op("act", lambda r=r: nc.scalar.activation(out=Rh[r][:, 0:ncol], in_=dotsB[:, 0:ncol], func=AF.Relu), [dotsB_b], [Rh_b[r]])
                else:
                    P.op("dve", lambda r=r: nc.vector.tensor_scalar(Rh[r][:, 0:ncol], dotsB[:, 0:ncol], 0.0, None, ALU.max), [dotsB_b], [Rh_b[r]])
                pend.append(h)
                if len(pend) > 1:
                    hh = pend.pop(0)
                    P.op("pe", lambda hh=hh: nc.tensor.matmul(accB[:, 0:ncol], diag[s2][:, hh, :], Rh[hh % NRH][:, 0:ncol], start=(hh == 0), stop=False),
                         [diag_b[s2], Rh_b[hh % NRH]], [accB_b], inc=False)
                yield 0.9 * ncol / 512
            for hh in pend:
                P.op("pe", lambda hh=hh: nc.tensor.matmul(accB[:, 0:ncol], diag[s2][:, hh, :], Rh[hh % NRH][:, 0:ncol], start=(hh == 0), stop=(hh == 7)),
                     [diag_b[s2], Rh_b[hh % NRH]], [accB_b], inc=(hh == 7))
            c_lo, c_hi = 0, ncol
            if s == 0:
                P.op("dve", lambda: nc.vector.tensor_tensor(score[:, 0:128], accB[:, 0:128], kbias[:], ALU.add), [accB_b, k1_b], [score_b])
                c_lo = 128
            if s == nslab - 1:
                P.op("dve", lambda: nc.vector.tensor_tensor(score[:, k0 + ncol - 128:k0 + ncol], accB[:, ncol - 128:ncol], tri[:], ALU.add),
                     [accB_b, k1_b], [score_b])
                c_hi = ncol - 128
            if c_hi > c_lo:
                P.op("act", lambda: nc.scalar.copy(score[:, k0 + c_lo:k0 + c_hi], accB[:, c_lo:c_hi]), [accB_b], [score_b])
            yield 0.6
        n_act = n_act_of(nk)
        n_dve = nk - n_act
        trb, cnb, sgb, ttb, ggb = colb["tr"], colb["cn"], colb["sg"], colb["tt"], colb["gg"]
        P.op("dve", lambda: nc.vector.memset(col(C_TR), 0.0), [], [trb])
        for it in range(N_ITER):
            step_next = R_RANGE / (2.0 ** (it + 1))
            if n_act > 0:
                P.op("act", lambda: nc.scalar.activation(out=junk_a[:, 0:n_act], in_=score[:, 0:n_act], func=AF.Sign, bias=col(C_TR), scale=-1.0,
                                                         accum_out=col(C_SG)), [score_b, trb], [junk_a_b, sgb])
            P.op("dve", lambda: nc.vector.tensor_scalar(junk_d[:, 0:n_dve], score[:, n_act:nk], col(C_TR), None, ALU.is_ge, ALU.add, accum_out=col(C_CN)),
                 [score_b, trb], [junk_d_b, cnb])
            if n_act > 0:
                P.op("dve", lambda: nc.vector.scalar_tensor_tensor(out=col(C_TT), in0=col(C_CN), scalar=2.0, in1=col(C_SG), op0=ALU.mult, op1=ALU.subtract),
                     [cnb, sgb], [ttb])
                thresh = 2.0 * TOPK - n_act - 1.0
            else:
                P.op("dve", lambda: nc.vector.tensor_copy(col(C_TT), col(C_CN)), [cnb], [ttb])
                thresh = TOPK - 0.5
            P.op("dve", lambda: nc.vector.tensor_scalar(col(C_GG), col(C_TT), thresh, 2.0 * step_next, ALU.is_ge, ALU.mult), [ttb], [ggb])
            P.op("dve", lambda: nc.vector.scalar_tensor_tensor(out=col(C_TR), in0=col(C_GG), scalar=-step_next, in1=col(C_TR), op0=ALU.add, op1=ALU.add),
                 [ggb, trb], [trb])
            yield 1.2 + max(n_act / 1200.0, n_dve / 960.0)
        step_last = R_RANGE / (2.0 ** N_ITER)
        thb = colb["thr0"] if s2 == 0 else colb["thr1"]
        P.op("dve", lambda: nc.vector.tensor_scalar(col(C_THR + s2), col(C_TR), -step_last, None, ALU.add), [trb], [thb])

    def fin_C(i):
        s2 = i % 2
        nk = (2 * i + 2) * 128
        thb = colb["thr0"] if s2 == 0 else colb["thr1"]
        P.op("dve", lambda: nc.vector.tensor_scalar(msk[:, 0:nk], score[:, 0:nk], col(C_THR + s2), None, ALU.is_lt), [score_b, thb], [msk_b])

    def gen_D(i):
        s3 = i % 3
        rows = slice(i * 128, (i + 1) * 128)
        nblk = 2 * i + 2
        oacc, oacc_b = acc, acc_b
        first_pv = [True, True]
        for s in range(nblk // AB):
            b0 = AB * s
            nb = AB
            ncol = nb * 128
            k0 = b0 * 128
            sl = s % 2
            P.dma("sp", KTs[sl][:, :, 0:ncol], KT_d[:, :, k0:k0 + ncol], [KT_b[b0 + j] for j in range(nb)], [KTs_b[sl]], KTs_b[sl])
            for j in range(nb):
                P.dma("sp", Vs[sl][:, j, :], V_d[b0 + j, :, :], [V_b[b0 + j]], [Vs_b[sl]], Vs_b[sl])
            P.op("pool", lambda: nc.gpsimd.tensor_scalar(mk[sl][:, 0:ncol], msk[:, k0:k0 + ncol], MNEG, 1.0, ALU.mult, ALU.mult), [msk_b], [mk_b[sl]])
            for j in range(nb):
                P.op("pe", lambda j=j: nc.tensor.transpose(tp[:, j * 128:(j + 1) * 128], mk[sl][:, j * 128:(j + 1) * 128], identb[:]),
                     [mk_b[sl], c_b], [tp_b], inc=(j == nb - 1))
            P.op("act", lambda: nc.scalar.copy(mkT[sl][:, 0:nb, :], tp[:, 0:ncol].rearrange("p (j t) -> p j t", j=nb)), [tp_b], [mkT_b[sl]])
            yield 0.6
            for j in range(nb):
                lbk = b0 + j
                near = None
                if lbk == nblk - 1:
                    near = bown
                elif lbk == nblk - 2:
                    near = both
                pk = (s * AB + j) % 2
                for par in range(2):
                    pr, prb = next_rotD()
                    prv = pr[:, :].rearrange("p (c t) -> p c t", c=4)
                    lo = 64 * par
                    for c in range(4):
                        P.op("pe", lambda c=c: nc.tensor.matmul(prv[:, c, :], KTs[sl][lo:lo + 64, c, j * 128:(j + 1) * 128],
                                                                qaT[s3][lo:lo + 64, c, :], start=(c == 0), stop=False),
                             [KTs_b[sl], qaT_b[s3]], [prb], inc=False)
                    if near is not None:
                        nv = near[:, :, :].rearrange("p (c two) t -> p c two t", two=2)[:, :, par, :]
                        P.op("pe", lambda: nc.tensor.matmul(prv, identb[:], nv, start=False, stop=False), [k1_b, c_b], [prb], inc=False)
                    mrep = mkT[sl][:, j, :].unsqueeze(1).to_broadcast([128, 4, 128])
                    P.op("pe", lambda: nc.tensor.matmul(prv, identb[:], mrep, start=False, stop=True), [mkT_b[sl], c_b], [prb])
                    P.op("act", lambda: nc.scalar.activation(out=PT[pk][:, par, :, :].rearrange("p c t -> p (c t)"), in_=pr[:, :], func=AF.Exp),
                         [prb], [PT_b[pk][par]])
                    yield 0.55
                for par in range(2):
                    ov = oacc[par][:, 0:260].rearrange("p (c d) -> p c d", c=4)
                    last_blk = (lbk == nblk - 1)
                    for c in range(4):
                        h = 2 * c + par
                        P.op("pe", lambda c=c, h=h: nc.tensor.matmul(ov[:, c, :], PT[pk][:, par, c, :], Vs[sl][:, j, h * 65:(h + 1) * 65],
                                                                    start=(first_pv[par] and c == 0), stop=(last_blk and c == 3)),
                             [PT_b[pk][par], Vs_b[sl]], [oacc_b[par]], inc=(c == 3))
                    first_pv[par] = False
                    yield 0.35
        for par in range(2):
            ov = oacc[par][:, 0:260].rearrange("p (c d) -> p c d", c=4)
            P.op("dve", lambda: nc.vector.reciprocal(rec[:, par * 4:(par + 1) * 4], ov[:, :, 64]), [oacc_b[par]], [rec_b])
            for c in range(4):
                h = 2 * c + par
                P.op("dve", lambda c=c, h=h: nc.vector.tensor_scalar(mix[s3][:, h * 64:(h + 1) * 64], ov[:, c, 0:64],
                                                                   rec[:, par * 4 + c:par * 4 + c + 1], None, ALU.mult),
                     [oacc_b[par], rec_b], [mix_b[s3]])
        P.dma("sp", MIX_d[rows, :], mix[s3][:], [mix_b[s3]], [MIX_b[i]], mix_b[s3])
        yield 1.0

    def interleave(gens):
        vt = [0.0] * len(gens)
        alive = [True] * len(gens)
        while any(alive):
            k = min((j for j in range(len(gens)) if alive[j]), key=lambda j: vt[j])
            try:
                vt[k] += next(gens[k])
            except StopIteration:
                alive[k] = False

    for r in range(NP + 2):
        gens = []
        if r - 2 >= 0:
            gens.append(gen_D(r - 2))
        if 0 <= r - 1 < NP:
            gens.append(gen_BC(r - 1))
        if r < NP:
            gens.append(gen_A(r))
        interleave(gens)
        if 0 <= r - 1 < NP:
            fin_C(r - 1)

    P.barrier()
    es1.close()
    es2 = ExitStack()
    def sb2(name, shape, dt):
        return es2.enter_context(nc.sbuf_tensor("s2_" + name, list(shape), dt))
    Wg = sb2("Wg", [128, 8, DFF], BF16); Wg_b = P.buf("Wg")
    Wu = sb2("Wu", [128, 8, DFF], BF16); Wu_b = P.buf("Wu")
    Wd = sb2("Wd", [128, NFC, D], BF16); Wd_b = P.buf("Wd")
    Wo = sb2("Wo", [128, 8, D], BF16); Wo_b = P.buf("Wo")
    stage2 = [sb2(f"stage2_{k}", [128, 8, 128], F32) for k in range(2)]
    stage2_b = [P.buf(f"stage2_{k}") for k in range(2)]
    gfin = sb2("gfin", [128, D], F32)
    gffn = sb2("gffn", [128, 8], F32)
    c3_b = P.buf("consts3")
    P.dma("sp", gfin[:], gfin_d[:, :], [], [c3_b], c3_b)
    P.dma("sp", gffn[:], gffn_d[:, :], [], [c3_b], c3_b)
    P.dma("pool", Wo[:], wo_d.rearrange("(c p) n -> p c n", p=128), [], [Wo_b], Wo_b)
    wd_v = wd_d.rearrange("(f p) n -> p f n", p=128)
    for f0 in range(0, NFC, 6):
        f1 = min(NFC, f0 + 6)
        P.dma("pool", Wd[:, f0:f1, :], wd_v[:, f0:f1, :], [], [Wd_b], Wd_b)
    load_weight_folded(Wg, Wg_b, wg_d, DFF, gffn, c3_b, stage2, stage2_b, 128)
    load_weight_folded(Wu, Wu_b, wu_d, DFF, gffn, c3_b, stage2, stage2_b, 128)

    NTOK = GB * 128
    x2t = [sb2(f"x2t{k}", [128, D], F32) for k in range(GB)]; x2t_b = [P.buf(f"x2t{k}") for k in range(GB)]
    mxt = sb2("mxt", [128, D], BF16); mxt_b = P.buf("mxt")
    mxT = sb2("mxT", [128, 8, 128], BF16); mxT_b = P.buf("mxT")
    x1 = [sb2(f"x1_{k}", [128, D], F32) for k in range(GB)]; x1_b = [P.buf(f"x1_{k}") for k in range(GB)]
    h2 = sb2("h2", [128, D], BF16); h2_b = P.buf("h2")
    h2T = sb2("h2T", [128, 8, NTOK], BF16); h2T_b = P.buf("h2T")
    uT = sb2("uT", [128, NFC, NTOK], BF16); uT_b = P.buf("uT")
    sg = [sb2(f"sg{k}", [128, NTOK], BF16) for k in range(2)]; sg_b = [P.buf(f"sg{k}") for k in range(2)]
    junk2, junk2_b = h2, h2_b
    xf = sb2("xf", [128, D], F32); xf_b = P.buf("xf")
    c2 = sb2("c2", [128, 16], F32)
    c2b = {k: P.buf("c2_" + k) for k in ("ssq", "tmp", "rs", "ssq2", "tmp2", "rs2")}

    for g in range(NP // GB):
        for k in range(GB):
            blk = g * GB + k
            rows = slice(blk * 128, (blk + 1) * 128)
            P.dma("sp", x2t[k][:], xo_d[rows, :], [], [x2t_b[k]], x2t_b[k])
            P.dma("sp", mxt[:], MIX_d[rows, :], [MIX_b[blk]], [mxt_b], mxt_b)
            for c in range(8):
                P.op("pe", lambda c=c: nc.tensor.transpose(tp[:, c * 128:(c + 1) * 128], mxt[:, c * 128:(c + 1) * 128], identb[:]),
                     [mxt_b, c_b], [tp_b], inc=(c == 7))
            P.op("dve", lambda: nc.vector.tensor_copy(mxT[:], tp[:, :].rearrange("p (c t) -> p c t", c=8)), [tp_b], [mxT_b])
            for n in range(2):
                pt, pb = next_mm()
                for c in range(8):
                    P.op("pe", lambda c=c, n=n, pt=pt: nc.tensor.matmul(pt[:, :], mxT[:, c, :], Wo[:, c, n * 512:(n + 1) * 512], start=(c == 0), stop=(c == 7)),
                         [mxT_b, Wo_b], [pb], inc=(c == 7))
                P.op("dve", lambda n=n, pt=pt, k=k: nc.vector.tensor_tensor(x1[k][:, n * 512:(n + 1) * 512], pt[:, :], x2t[k][:, n * 512:(n + 1) * 512], ALU.add),
                     [pb, x2t_b[k]], [x1_b[k]])
            P.op("act", lambda k=k: nc.scalar.activation(out=junk2[:], in_=x1[k][:], func=AF.Square, accum_out=c2[:, 0:1]), [x1_b[k]], [junk2_b, c2b["ssq"]])
            rstd_from_ssq(c2[:, 0:1], c2b["ssq"], c2[:, 2:3], c2b["rs"], D, 1, c2[:, 1:2], c2b["tmp"])
            P.op("dve", lambda k=k: nc.vector.tensor_scalar(h2[:], x1[k][:], c2[:, 2:3], None, ALU.mult), [x1_b[k], c2b["rs"]], [h2_b])
            for c in range(8):
                P.op("pe", lambda c=c: nc.tensor.transpose(tp[:, c * 128:(c + 1) * 128], h2[:, c * 128:(c + 1) * 128], identb[:]),
                     [h2_b, c_b], [tp_b], inc=(c == 7))
            P.op("act", lambda k=k: nc.scalar.copy(h2T[:, :, k * 128:(k + 1) * 128], tp[:, :].rearrange("p (c t) -> p c t", c=8)), [tp_b], [h2T_b])
        for f in range(NFC):
            pg, pgb = next_mm()
            for c in range(8):
                P.op("pe", lambda c=c, f=f, pg=pg: nc.tensor.matmul(pg[:, 0:NTOK], Wg[:, c, f * 128:(f + 1) * 128], h2T[:, c, :], start=(c == 0), stop=(c == 7)),
                     [Wg_b, h2T_b], [pgb], inc=(c == 7))
            pu, pub = next_mm()
            for c in range(8):
                P.op("pe", lambda c=c, f=f, pu=pu: nc.tensor.matmul(pu[:, 0:NTOK], Wu[:, c, f * 128:(f + 1) * 128], h2T[:, c, :], start=(c == 0), stop=(c == 7)),
                     [Wu_b, h2T_b], [pub], inc=(c == 7))
            P.op("act", lambda f=f, pg=pg: nc.scalar.activation(out=sg[f % 2][:], in_=pg[:, 0:NTOK], func=AF.Silu), [pgb], [sg_b[f % 2]])
            P.op("dve", lambda f=f, pu=pu: nc.vector.tensor_tensor(uT[:, f, :], pu[:, 0:NTOK], sg[f % 2][:], ALU.mult), [pub, sg_b[f % 2]], [uT_b])
        for k in range(GB):
            blk = g * GB + k
            rows = slice(blk * 128, (blk + 1) * 128)
            for n in range(2):
                pt, pb = next_mm()
                for f in range(NFC):
                    P.op("pe", lambda f=f, n=n, pt=pt, k=k: nc.tensor.matmul(pt[:, :], uT[:, f, k * 128:(k + 1) * 128], Wd[:, f, n * 512:(n + 1) * 512],
                                                                             start=(f == 0), stop=(f == NFC - 1)),
                         [uT_b, Wd_b], [pb], inc=(f == NFC - 1))
                P.op("dve", lambda n=n, pt=pt, k=k: nc.vector.tensor_tensor(xf[:, n * 512:(n + 1) * 512], pt[:, :], x1[k][:, n * 512:(n + 1) * 512], ALU.add),
                     [pb, x1_b[k]], [xf_b])
            P.op("act", lambda: nc.scalar.activation(out=junk2[:], in_=xf[:], func=AF.Square, accum_out=c2[:, 4:5]), [xf_b], [junk2_b, c2b["ssq2"]])
            rstd_from_ssq(c2[:, 4:5], c2b["ssq2"], c2[:, 6:7], c2b["rs2"], D, 1, c2[:, 5:6], c2b["tmp2"])
            o_t, o_b = x2t[k], x2t_b[k]
            P.op("dve", lambda o_t=o_t: nc.vector.scalar_tensor_tensor(out=o_t[:], in0=xf[:], scalar=c2[:, 6:7], in1=gfin[:], op0=ALU.mult, op1=ALU.mult),
                 [xf_b, c2b["rs2"], c3_b], [o_b])
            P.dma("sp", out_d[rows, :], o_t[:], [o_b], [], o_b)
    P.barrier()
    es2.close()
    esp.close()
    es0.close()
    build.stats = dict(ninst=P.ninst, nwait=P.nwait, nsem=P.nsem)
    return nc


def _rel_bucket(n):
    n = np.maximum(n, 0).astype(np.int32)
    max_exact = 16
    nf = np.maximum(n, max_exact).astype(np.float32)
    large = max_exact + (np.log(nf / np.float32(max_exact)) / np.float32(math.log(128 / 16)) * np.float32(16)).astype(np.int32)
    large = np.minimum(large, 31)
    return np.where(n < max_exact, n, large)


def prep_inputs(inputs, NP):
    SEQ = 256 * NP
    x = np.asarray(inputs["x"], dtype=np.float32)
    B = x.shape[0]
    assert x.shape[1] == SEQ
    w_in = np.asarray(inputs["w_in"], dtype=np.float32)[0]
    o = np.cumsum([0, 512, 512, 512, 512, 64, 8, 512, 512, 512, 512])
    qa, ka, va, qi, ki, wi, qb, kb, vb, gb = [w_in[:, o[k]:o[k + 1]] for k in range(10)]
    wk = np.ascontiguousarray(np.concatenate([ka, va, kb, vb, ki], axis=1))
    wq = np.ascontiguousarray(np.concatenate([qa, qi, qb, gb, wi], axis=1))
    f32 = lambda a: np.ascontiguousarray(np.asarray(a, dtype=np.float32))
    col8 = lambda g: f32(np.asarray(g, dtype=np.float32).reshape(8, 128).T)
    bc = lambda v: f32(np.broadcast_to(np.asarray(v, dtype=np.float32).reshape(1, -1), (128, np.asarray(v).size)))
    rel_bias = np.asarray(inputs["rel_bias"], dtype=np.float32)
    kk = np.arange(128)[:, None]
    qq = np.arange(128)[None, :]
    bkt_own = _rel_bucket(qq - kk)
    bkt_oth = _rel_bucket(qq + 128 - kk)
    bown = np.where((qq >= kk)[:, None, :], rel_bias[bkt_own].transpose(0, 2, 1), 0.0)
    both = rel_bias[bkt_oth].transpose(0, 2, 1)
    theta = (1.0 / (np.float32(10000.0) ** np.linspace(0.0, 1.0, 64, dtype=np.float32))).astype(np.float32)
    pos = np.arange(SEQ, dtype=np.float32)
    ang = (pos[:, None] * theta[None, :]).astype(np.float32)
    cos_all = np.cos(ang.astype(np.float64)).astype(np.float32)
    sin_all = np.sin(ang.astype(np.float64)).astype(np.float32)
    gamma = 1.0 - 2.0 ** (-5.0 - np.arange(4, dtype=np.float64))
    ii = np.arange(128, dtype=np.float64)[:, None]
    decq = (gamma[None, :] ** (ii + 1.0)).astype(np.float32)
    deck = ((gamma[None, :] ** (-(ii + 1.0))) * (128.0 ** -0.5)).astype(np.float32)
    gcv = np.broadcast_to((gamma ** 128.0).astype(np.float32)[None, :], (128, 4))
    tri = np.where(np.arange(128)[None, :] <= np.arange(128)[:, None], 0.0, NEG).astype(np.float32)
    trir = (np.arange(128)[None, :] >= np.arange(128)[:, None]).astype(np.float32)
    shared = dict(
        wk=wk, wq=wq, wo=f32(inputs["w_out"][0]), wg=f32(inputs["w_gate"][0]), wu=f32(inputs["w_up"][0]), wd=f32(inputs["w_down"][0]),
        gmix=col8(inputs["norm_mix_g"][0]), gffn=col8(inputs["norm_ffn_g"][0]), gfin=bc(inputs["norm_final_g"]),
        lng=bc(inputs["idx_k_ln_g"][0]), lnb=bc(inputs["idx_k_ln_b"][0]), gn=bc(inputs["ret_gn_g"][0]), b31=bc(rel_bias[31]),
        bown=f32(bown), both=f32(both), decq=f32(decq), deck=f32(deck), gc=f32(gcv),
        ident=np.eye(128, dtype=np.float32), tri=tri, trir=f32(trir),
    )
    in_maps = []
    for core in range(2 * B):
        b, p = core // 2, core % 2
        xb = x[b].reshape(2 * NP, 128, D)
        own_ids = [2 * i + p for i in range(NP)]
        oth_ids = [2 * i - 1 + p for i in range(NP)]
        xo = f32(xb[own_ids].reshape(NP * 128, D))
        xt = np.zeros((NP, 128, D), np.float32)
        cost = np.zeros((NP, 128, 64), np.float32)
        sint = np.zeros((NP, 128, 64), np.float32)
        cb = cos_all.reshape(2 * NP, 128, 64)
        sbk = sin_all.reshape(2 * NP, 128, 64)
        for i, gidx in enumerate(oth_ids):
            if gidx >= 0:
                xt[i] = xb[gidx]
                cost[i] = cb[gidx]
                sint[i] = sbk[gidx]
        kbias = np.zeros((128, 128), np.float32) if p == 1 else np.full((128, 128), NEG, np.float32)
        m = dict(shared)
        m.update(xo=xo, xt=f32(xt.reshape(NP * 128, D)), coso=f32(cb[own_ids].reshape(NP * 128, 64)), sino=f32(sbk[own_ids].reshape(NP * 128, 64)),
                 cost=f32(cost.reshape(NP * 128, 64)), sint=f32(sint.reshape(NP * 128, 64)), kbias=kbias)
        in_maps.append(m)
    return in_maps


def assemble(results, B, NP):
    out = np.zeros((B, 2 * NP, 128, D), np.float32)
    for core in range(2 * B):
        b, p = core // 2, core % 2
        r = np.asarray(results[core]["out"]).reshape(NP, 128, D)
        for i in range(NP):
            out[b, 2 * i + p] = r[i]
    return out.reshape(B, 2 * NP * 128, D)


def kernel(x, norm_mix_g, w_in, idx_k_ln_g, idx_k_ln_b, rel_bias, ret_gn_g, w_out, norm_ffn_g, w_gate, w_up, w_down, norm_final_g):
    inputs = dict(x=x, norm_mix_g=norm_mix_g, w_in=w_in, idx_k_ln_g=idx_k_ln_g, idx_k_ln_b=idx_k_ln_b, rel_bias=rel_bias,
                  ret_gn_g=ret_gn_g, w_out=w_out, norm_ffn_g=norm_ffn_g, w_gate=w_gate, w_up=w_up, w_down=w_down,
                  norm_final_g=norm_final_g)
    inputs = {k: np.asarray(v) for k, v in inputs.items()}
    B, SEQ = inputs["x"].shape[0], inputs["x"].shape[1]
    NP = SEQ // 256
    TOPK = min(256, SEQ // 4)
    nc = build(NP, TOPK)
    in_maps = prep_inputs(inputs, NP)
    res = run_bass_kernel_spmd(nc, in_maps, core_ids=list(range(2 * B)))
    return assemble(res.results, B, NP)
```
